# Optimizing a Trainium2 kernel written in Bass

```python
import jax, jax.numpy as jnp
from jax import lax
import numpy as np

D_MODEL = 1024
BATCH = 32
SEQ = 2048
DEPTH = 4

HEAD_DIM = 64
N_HEADS_TOTAL = D_MODEL // HEAD_DIM
N_HEADS_A = N_HEADS_TOTAL // 4
N_HEADS_B = N_HEADS_TOTAL // 4
N_HEADS_C = N_HEADS_TOTAL // 2
N_KV_C = N_HEADS_C // 4
N_IDX_HEADS = 4
IDX_DIM = HEAD_DIM
TOPK_MAX = 256
WINDOW = 128
BLOCK = 128
ROPE_THETA = 10000.0
NORM_EPS = 1e-6
MIX_WIDTH = N_HEADS_TOTAL * HEAD_DIM
D_FF = -(-8 * D_MODEL // (3 * 256)) * 256
_COLS = (N_HEADS_A * HEAD_DIM, HEAD_DIM, HEAD_DIM,
         N_IDX_HEADS * IDX_DIM, IDX_DIM, N_IDX_HEADS,
         N_HEADS_B * HEAD_DIM, N_HEADS_B * HEAD_DIM, N_HEADS_B * HEAD_DIM,
         N_HEADS_C * HEAD_DIM, N_KV_C * HEAD_DIM, N_KV_C * HEAD_DIM)
IN_COLS = sum(_COLS)

kernel_name = "hymba_style_dsa_stickbreak_swa_hybrid"


def _rms(x, g):
    xf = x.astype(jnp.float32)
    y = xf * lax.rsqrt(jnp.mean(xf * xf, axis=-1, keepdims=True) + NORM_EPS)
    return (y * g.astype(jnp.float32)).astype(x.dtype)


def _rope_tables(L):
    inv = 1.0 / (ROPE_THETA ** (jnp.arange(0, HEAD_DIM, 2, dtype=jnp.float32) / HEAD_DIM))
    ang = jnp.arange(L, dtype=jnp.float32)[:, None] * inv[None, :]
    return jnp.cos(ang), jnp.sin(ang)


def _rope(x, cos, sin):
    c = cos[None, :, None, :].astype(x.dtype)
    s = sin[None, :, None, :].astype(x.dtype)
    x1, x2 = jnp.split(x, 2, axis=-1)
    return jnp.concatenate([x1 * c - x2 * s, x2 * c + x1 * s], axis=-1)


def _blocks(a):
    B, L = a.shape[:2]
    return jnp.moveaxis(a.reshape((B, L // BLOCK, BLOCK) + a.shape[2:]), 1, 0)


def _unblocks(a):
    nb, B = a.shape[:2]
    return jnp.moveaxis(a, 0, 1).reshape((B, nb * BLOCK) + a.shape[3:])


def _split_points():
    pts, acc = [], 0
    for w in _COLS[:-1]:
        acc += w
        pts.append(acc)
    return pts


def _dsa(q, k, v, qi, ki, wi):
    B, L, H, D = q.shape
    n_sel = min(TOPK_MAX, L // 4)
    scale = D ** -0.5
    idx_scale = (N_IDX_HEADS * IDX_DIM) ** -0.5
    kpos = jnp.arange(L)

    def one_block(inp):
        qn, qin, win, n = inp
        tq = n * BLOCK + jnp.arange(BLOCK)
        rel = jax.nn.relu(jnp.einsum('bqhd,bsd->bqhs', qin, ki).astype(jnp.float32))
        score = jnp.einsum('bqh,bqhs->bqs', win.astype(jnp.float32), rel) * idx_scale
        causal = kpos[None, :] <= tq[:, None]
        score = jnp.where(causal[None], score, -jnp.inf)
        _, sel = lax.top_k(score, n_sel)
        valid = sel <= tq[None, :, None]
        ks = jax.vmap(lambda a, i: a[i])(k, sel)
        vs = jax.vmap(lambda a, i: a[i])(v, sel)
        s = jnp.einsum('bqhd,bqkd->bhqk', qn, ks).astype(jnp.float32) * scale
        s = jnp.where(valid[:, None], s, -jnp.inf)
        p = jax.nn.softmax(s, axis=-1)
        return jnp.einsum('bhqk,bqkd->bqhd', p.astype(vs.dtype), vs)

    out = lax.map(one_block, (_blocks(q), _blocks(qi), _blocks(wi), jnp.arange(L // BLOCK)))
    return _unblocks(out)


def _stick_breaking(q, k, v):
    B, L, H, D = q.shape
    scale = D ** -0.5
    kpos = jnp.arange(L)

    def one_block(inp):
        qn, n = inp
        tq = n * BLOCK + jnp.arange(BLOCK)
        strict = (kpos[None, :] < tq[:, None])[None, None]
        z = jnp.einsum('bqhd,bshd->bhqs', qn, k).astype(jnp.float32) * scale
        log_keep = jnp.where(strict, jax.nn.log_sigmoid(-z), 0.0)
        log_between = lax.cumsum(log_keep, axis=3, reverse=True) - log_keep
        a = jnp.where(strict, jnp.exp(jax.nn.log_sigmoid(z) + log_between), 0.0)
        return jnp.einsum('bhqs,bshd->bqhd', a.astype(v.dtype), v)

    out = lax.map(one_block, (_blocks(q), jnp.arange(L // BLOCK)))
    return _unblocks(out)


def _swa_sinks(q, k, v, sinks):
    B, L, H, D = q.shape
    G = H // N_KV_C
    nb = L // BLOCK
    scale = D ** -0.5
    pad = ((0, 0), (1, 0), (0, 0), (0, 0), (0, 0))
    kb = k.reshape(B, nb, BLOCK, N_KV_C, D)
    vb = v.reshape(B, nb, BLOCK, N_KV_C, D)
    kk = jnp.concatenate([jnp.pad(kb, pad)[:, :-1], kb], axis=2)
    vv = jnp.concatenate([jnp.pad(vb, pad)[:, :-1], vb], axis=2)
    qg = q.reshape(B, nb, BLOCK, N_KV_C, G, D)
    kloc = jnp.arange(2 * BLOCK)
    diff = (jnp.arange(BLOCK)[:, None] + BLOCK) - kloc[None, :]
    band = (diff >= 0) & (diff < WINDOW)
    sink = sinks.astype(jnp.float32).reshape(N_KV_C, G)[None, :, :, None, None]

    def one_block(inp):
        qn, kn, vn, n = inp
        s = jnp.einsum('bqkgd,bskd->bkgqs', qn, kn).astype(jnp.float32) * scale
        mask = band & ((n > 0) | (kloc >= BLOCK))[None, :]
        s = jnp.where(mask, s, -jnp.inf)
        m = jnp.maximum(jnp.max(s, axis=-1, keepdims=True), sink)
        e = jnp.exp(s - m)
        p = e / (jnp.sum(e, axis=-1, keepdims=True) + jnp.exp(sink - m))
        return jnp.einsum('bkgqs,bskd->bqkgd', p.astype(vn.dtype), vn)

    out = lax.map(one_block, (jnp.moveaxis(qg, 1, 0), jnp.moveaxis(kk, 1, 0),
                              jnp.moveaxis(vv, 1, 0), jnp.arange(nb)))
    return _unblocks(out).reshape(B, L, H, D)


def _mixing(h, w_in, qn_a, kn_a, qn_c, kn_c, sinks, g_out, w_o, cos, sin):
    B, L, _ = h.shape
    p = h @ w_in
    qa, ka, va, qi, ki, wi, qb, kb, vb, qc, kc, vc = jnp.split(p, _split_points(), axis=-1)
    qa = _rope(_rms(qa.reshape(B, L, N_HEADS_A, HEAD_DIM), qn_a), cos, sin)
    ka = _rope(_rms(ka.reshape(B, L, 1, HEAD_DIM), kn_a), cos, sin)[:, :, 0]
    qi = _rope(qi.reshape(B, L, N_IDX_HEADS, IDX_DIM), cos, sin)
    ki = _rope(ki.reshape(B, L, 1, IDX_DIM), cos, sin)[:, :, 0]
    oa = _dsa(qa, ka, va, qi, ki, wi)
    ob = _stick_breaking(qb.reshape(B, L, N_HEADS_B, HEAD_DIM),
                         kb.reshape(B, L, N_HEADS_B, HEAD_DIM),
                         vb.reshape(B, L, N_HEADS_B, HEAD_DIM))
    qc = _rope(_rms(qc.reshape(B, L, N_HEADS_C, HEAD_DIM), qn_c), cos, sin)
    kc = _rope(_rms(kc.reshape(B, L, N_KV_C, HEAD_DIM), kn_c), cos, sin)
    oc = _swa_sinks(qc, kc, vc.reshape(B, L, N_KV_C, HEAD_DIM), sinks)
    o = jnp.concatenate([oa, ob, oc], axis=2)
    o = _rms(o, g_out).reshape(B, L, MIX_WIDTH)
    return o @ w_o


def _swiglu(h, w_gate, w_up, w_down):
    return (jax.nn.silu(h @ w_gate) * (h @ w_up)) @ w_down


def setup_inputs(seed: int = 0) -> dict:
    key = jax.random.key(seed)
    ks = jax.random.split(key, 20)
    f = jnp.float32
    D = D_MODEL

    def nrm(k, shape, scale):
        return jax.random.normal(k, shape, f) * scale

    def gain(k, shape):
        return 1.0 + 0.02 * jax.random.normal(k, shape, f)

    return {
        "x": nrm(ks[0], (BATCH, SEQ, D), 1.0),
        "c": nrm(ks[1], (BATCH, D), 1.0),
        "ln1": gain(ks[2], (DEPTH, D)),
        "ln2": gain(ks[3], (DEPTH, D)),
        "w_mod": nrm(ks[4], (DEPTH, D, 6 * D), 0.5 * D ** -0.5),
        "b_mod": nrm(ks[5], (DEPTH, 6 * D), 0.02),
        "w_in": nrm(ks[6], (DEPTH, D, IN_COLS), D ** -0.5),
        "qn_a": gain(ks[7], (DEPTH, HEAD_DIM)),
        "kn_a": gain(ks[8], (DEPTH, HEAD_DIM)),
        "qn_c": gain(ks[9], (DEPTH, HEAD_DIM)),
        "kn_c": gain(ks[10], (DEPTH, HEAD_DIM)),
        "sinks": nrm(ks[11], (DEPTH, N_HEADS_C), 0.5),
        "g_out": gain(ks[12], (DEPTH, N_HEADS_TOTAL, HEAD_DIM)),
        "w_o": nrm(ks[13], (DEPTH, MIX_WIDTH, D), MIX_WIDTH ** -0.5),
        "w_gate": nrm(ks[14], (DEPTH, D, D_FF), D ** -0.5),
        "w_up": nrm(ks[15], (DEPTH, D, D_FF), D ** -0.5),
        "w_down": nrm(ks[16], (DEPTH, D_FF, D), D_FF ** -0.5),
    }


def reference(x, c, ln1, ln2, w_mod, b_mod, w_in, qn_a, kn_a, qn_c, kn_c, sinks, g_out, w_o,
              w_gate, w_up, w_down):
    L = x.shape[1]
    cos, sin = _rope_tables(L)
    c_act = jax.nn.silu(c)
    for l in range(DEPTH):
        mod = c_act @ w_mod[l] + b_mod[l]
        sh1, sc1, g1, sh2, sc2, g2 = [m[:, None, :] for m in jnp.split(mod, 6, axis=-1)]
        h = _rms(x, ln1[l]) * (1 + sc1) + sh1
        x = x + g1 * _mixing(h, w_in[l], qn_a[l], kn_a[l], qn_c[l], kn_c[l], sinks[l],
                             g_out[l], w_o[l], cos, sin)
        h = _rms(x, ln2[l]) * (1 + sc2) + sh2
        x = x + g2 * _swiglu(h, w_gate[l], w_up[l], w_down[l])
    return x
```

```python
import contextlib
import numpy as np
import concourse.bass as bass
import concourse.mybir as mybir
from concourse.bass_utils import run_bass_kernel_spmd

F32 = mybir.dt.float32
BF16 = mybir.dt.bfloat16
U32 = mybir.dt.uint32
FP8 = mybir.dt.float8e4
U8 = mybir.dt.uint8
AF = mybir.ActivationFunctionType
ALU = mybir.AluOpType
AX = mybir.AxisListType

D = 1024
DFF = 2816
NFF = DFF // 128
INC = 2244
HD = 64
EPS = 1e-6
NEG = -1.0e30
NIT = 20
import os
STOP = int(os.environ.get('KSTOP', '9'))
SKIP = os.environ.get('KSKIP', '')
O_QA, O_KA, O_VA, O_QI, O_KI, O_WI, O_QB, O_KB, O_VB, O_QC, O_KC, O_VC = (
    0, 256, 320, 384, 640, 704, 708, 964, 1220, 1476, 1988, 2116)


class Buf:
    __slots__ = ("name", "w", "r", "dsem", "excl")

    def __init__(self, name, excl=False):
        self.name = name
        self.excl = excl
        self.w = None
        self.r = []
        self.dsem = None


class V:
    __slots__ = ("ap", "buf")

    def __init__(self, ap, buf):
        self.ap = ap
        self.buf = buf

    def __getitem__(self, idx):
        return V(self.ap[idx], self.buf)


class Prog:
    ENG = ("pe", "dve", "act", "pool", "sp")

    def __init__(self, nc, es):
        self.nc = nc
        self.es = es
        self.ops = {e: [] for e in self.ENG}
        self.cnt = {}
        self.seen = {e: {} for e in self.ENG}
        self.sems = {}
        for e in ("pe", "dve", "act", "pool"):
            self.sems[e] = es.enter_context(nc.semaphore("S_" + e))
            self.cnt[e] = 0
        self.ndsem = 0
        self.nt = 0
        self.out_waits = []

    def sb(self, shape, dt, name=None):
        self.nt += 1
        name = name or f"t{self.nt}"
        t = self.es.enter_context(self.nc.sbuf_tensor(name + f"_{self.nt}", list(shape), dt))
        return V(t[:], Buf(name))

    def ps(self, name):
        self.nt += 1
        t = self.es.enter_context(self.nc.psum_tensor(name + f"_{self.nt}", [128, 512], F32))
        return V(t[:], Buf(name, excl=True))

    def arena_init(self, nbytes):
        t = self.es.enter_context(self.nc.sbuf_tensor("arena", [128, nbytes // 4], F32))
        self.arena = t[:]
        self.arena_n = nbytes
        self.arena_off = 0
        self.arena_max = 0

    def arena_reset(self):
        self.arena_off = 0

    def ar(self, shape, dt, name):
        n = 1
        for d in shape[1:]:
            n *= d
        esz = {F32: 4, BF16: 2, U32: 4, FP8: 1, U8: 1}[dt]
        nb = (n * esz + 3) // 4 * 4
        off = self.arena_off
        self.arena_off += nb
        self.arena_max = max(self.arena_max, self.arena_off)
        assert self.arena_off <= self.arena_n, (name, self.arena_off, self.arena_n)
        ap = self.arena[:, off // 4:(off + nb) // 4]
        if dt != F32:
            ap = ap.bitcast(dt)
        ap = ap[:, 0:n]
        if len(shape) == 3:
            ap = ap.rearrange("p (a b) -> p a b", b=shape[2])
        elif len(shape) == 4:
            ap = ap.rearrange("p (a b c) -> p a b c", b=shape[2], c=shape[3])
        return V(ap, Buf(name))

    def dram(self, name, shape, dt):
        t = self.nc.dram_tensor(name, list(shape), dt, kind="Internal")
        return V(t.ap(), Buf(name))

    def dsem(self, buf):
        if buf.dsem is None:
            key = f"d{self.ndsem}"
            self.ndsem += 1
            self.sems[key] = self.es.enter_context(self.nc.semaphore("D_" + key))
            self.cnt[key] = 0
            buf.dsem = key
        return buf.dsem

    def _deps(self, eng, reads, writes):
        need = {}
        for b in reads:
            if b.w is not None:
                k, v = b.w
                need[k] = max(need.get(k, 0), v)
            if b.excl:
                for (k, v) in b.r:
                    if k != eng:
                        need[k] = max(need.get(k, 0), v)
        for b in writes:
            if b.w is not None:
                k, v = b.w
                need[k] = max(need.get(k, 0), v)
            for (k, v) in b.r:
                need[k] = max(need.get(k, 0), v)
        waits = []
        seen = self.seen[eng]
        for k, v in need.items():
            if k == eng:
                if eng == "pe":
                    continue
            if seen.get(k, 0) >= v:
                continue
            seen[k] = v
            waits.append((k, v))
        return waits

    def _mark(self, key, val, reads, writes):
        for b in reads:
            b.r.append((key, val))
            if len(b.r) > 24:
                m = {}
                for (k, v) in b.r:
                    m[k] = max(m.get(k, 0), v)
                b.r = list(m.items())
        for b in writes:
            b.w = (key, val)
            b.r = []

    def op(self, eng, fn, reads, writes):
        rb = [x.buf for x in reads]
        wb = [x.buf for x in writes]
        waits = self._deps(eng, rb, wb)
        self.cnt[eng] += 1
        self._mark(eng, self.cnt[eng], rb, wb)
        self.ops[eng].append((fn, waits, (eng, 1)))

    def dma(self, q, out, in_, out_is_ext=False):
        rb = [in_.buf]
        wb = [out.buf]
        waits = self._deps(q, rb, wb)
        key = self.dsem(out.buf)
        self.cnt[key] += 16
        self._mark(key, self.cnt[key], rb, wb)
        oa, ia = out.ap, in_.ap
        self.ops[q].append((lambda e: e.dma_start(out=oa, in_=ia), waits, (key, 16)))
        if out_is_ext:
            self.out_waits.append((key, self.cnt[key]))

    def barrier(self):
        for e in self.ENG:
            waits = []
            for k, v in self.cnt.items():
                if k == e or v == 0:
                    continue
                if self.seen[e].get(k, 0) >= v:
                    continue
                self.seen[e][k] = v
                waits.append((k, v))
            if waits:
                self.ops[e].append((None, waits, None))

    def emit(self, block):
        nc = self.nc
        engs = {"pe": (block.tensor, None), "dve": (block.vector, None), "act": (block.scalar, None),
                "pool": (block.gpsimd, None), "sp": (block.sync, None)}
        self.ops["sp"].append((None, list(self.out_waits), None))
        for name in self.ENG:
            ops = self.ops[name]
            sems = self.sems

            def body(e, ops=ops):
                for (fn, waits, inc) in ops:
                    for (k, v) in waits:
                        e.wait_ge(sems[k], v)
                    if fn is not None:
                        ins = fn(e)
                        ins.then_inc(sems[inc[0]], inc[1])
            engs[name][0](body)

    def mm(self, out, lhsT, rhs, start=True, stop=True):
        o, l, r = out.ap, lhsT.ap, rhs.ap
        self.op("pe", lambda e: e.matmul(o, lhsT=l, rhs=r, start=start, stop=stop, skip_group_check=True),
                [lhsT, rhs], [out])

    def tr(self, out, in_, ident):
        o, i, d = out.ap, in_.ap, ident.ap
        self.op("pe", lambda e: e.transpose(o, i, d), [in_, ident], [out])

    def act(self, out, in_, func, scale=1.0, bias=None, extra=()):
        o, i = out.ap, in_.ap
        rd = [in_] + list(extra)
        sc = scale.ap if isinstance(scale, V) else scale
        if isinstance(scale, V):
            rd.append(scale)
        if isinstance(bias, V):
            rd.append(bias)
            bi = bias.ap
            self.op("act", lambda e: e.activation(out=o, in_=i, func=func, bias=bi, scale=sc), rd, [out])
        elif bias is None:
            self.op("act", lambda e: e.activation(out=o, in_=i, func=func, scale=sc), rd, [out])
        else:
            self.op("act", lambda e: e.activation(out=o, in_=i, func=func, bias=bias, scale=sc), rd, [out])

    def ts(self, eng, out, in0, s1, op0, s2=None, op1=None, accum=None):
        o, i = out.ap, in0.ap
        rd = [in0]
        a1 = s1.ap if isinstance(s1, V) else s1
        a2 = s2.ap if isinstance(s2, V) else s2
        if isinstance(s1, V):
            rd.append(s1)
        if isinstance(s2, V):
            rd.append(s2)
        wr = [out]
        kw = {}
        if op1 is not None:
            kw["op1"] = op1
        if accum is not None:
            kw["accum_out"] = accum.ap
            wr.append(accum)
        self.op(eng, lambda e: e.tensor_scalar(out=o, in0=i, scalar1=a1, scalar2=a2, op0=op0, **kw), rd, wr)

    def tt(self, eng, out, in0, in1, op):
        o, a, b = out.ap, in0.ap, in1.ap
        self.op(eng, lambda e: e.tensor_tensor(out=o, in0=a, in1=b, op=op), [in0, in1], [out])

    def stt(self, out, in0, scalar, in1, op0, op1):
        o, a, b = out.ap, in0.ap, in1.ap
        rd = [in0, in1]
        s = scalar.ap if isinstance(scalar, V) else scalar
        if isinstance(scalar, V):
            rd.append(scalar)
        self.op("dve", lambda e: e.scalar_tensor_tensor(out=o, in0=a, scalar=s, in1=b, op0=op0, op1=op1), rd, [out])

    def copy(self, eng, out, in_):
        o, i = out.ap, in_.ap
        if eng == "act":
            self.op("act", lambda e: e.activation(out=o, in_=i, func=AF.Copy), [in_], [out])
        else:
            self.op(eng, lambda e: e.tensor_copy(out=o, in_=i), [in_], [out])

    def memset(self, eng, out, val):
        o = out.ap
        self.op(eng, lambda e: e.memset(o, val), [], [out])

    def recip(self, out, in_):
        o, i = out.ap, in_.ap
        self.op("dve", lambda e: e.reciprocal(out=o, in_=i), [in_], [out])

    def reduce(self, out, in_, op):
        o, i = out.ap, in_.ap
        self.op("dve", lambda e: e.tensor_reduce(out=o, in_=i, axis=AX.X, op=op), [in_], [out])


def build_program(L, NBC, layers):
    NB = L // 128
    NT = L // 512
    NSEL = min(256, L // 4)
    NL = 4
    nc = bass.Bass("TRN2", target_bir_lowering=False)
    es = contextlib.ExitStack()
    with es:
        P = Prog(nc, es)

        def ext(name, shape, dt=F32, out=False):
            t = nc.dram_tensor(name, list(shape), dt, kind="ExternalOutput" if out else "ExternalInput")
            return V(t.ap(), Buf(name))

        x_d = ext("x", [NBC, L, D])
        y_d = ext("y", [NBC, L, D], out=True)
        cT_d = ext("cT", [128, 8, NBC])
        ln1_d = ext("ln1T", [128, NL, 8])
        ln2_d = ext("ln2T", [128, NL, 8])
        bmod_d = ext("bmodT", [128, NL, 48])
        wmod_d = ext("w_mod", [NL, D, 6 * D])
        win_d = ext("w_in", [NL, D, INC])
        wo_d = ext("w_o", [NL, D, D])
        wg_d = ext("w_gate", [NL, D, DFF])
        wu_d = ext("w_up", [NL, D, DFF])
        wd_d = ext("w_down", [NL, DFF, D])
        gains_d = ext("gainsB", [128, NL, 4, HD])
        sinks_d = ext("sinksT", [128, NL, 4])
        gout_d = ext("goutT", [128, NL, 8])
        cos_d = ext("cosT", [128, NB, 32])
        sin_d = ext("sinT", [128, NB, 32])
        cst_d = ext("consts", [128, 6, 128])
        neg_d = ext("negm", [128, 128])

        win_b = P.dram("win_b", [NL, D, INC], BF16)
        wo_b = P.dram("wo_b", [NL, D, D], BF16)
        wg_b = P.dram("wg_b", [NL, D, DFF], BF16)
        wu_b = P.dram("wu_b", [NL, D, DFF], BF16)
        wd_b = P.dram("wd_b", [NL, DFF, D], BF16)
        for (dst, src, rows) in ((win_b, win_d, D), (wo_b, wo_d, D), (wg_b, wg_d, D), (wu_b, wu_d, D), (wd_b, wd_d, DFF)):
            for l in (layers if not os.environ.get('KNOCONV') else []):
                for r0 in range(0, rows, 512):
                    r1 = min(rows, r0 + 512)
                    P.dma("pool", V(dst.ap[l, r0:r1, :], dst.buf), V(src.ap[l, r0:r1, :], src.buf))

        cst = P.sb([128, 6, 128], BF16, "cst")
        P.dma("pool", cst, cst_d)
        ident, m_le, m_lt, m_gt, tinc, bones = (cst[:, i, :] for i in range(6))
        ident_f = P.sb([128, 128], F32, "identf")
        P.dma("sp", ident_f, V(cst_d.ap[:, 0, :], cst_d.buf))
        negm = P.sb([128, 128], F32, "negm")
        P.dma("sp", negm, neg_d)
        cosT = P.sb([128, NB, 32], F32, "cos")
        sinT = P.sb([128, NB, 32], F32, "sin")
        P.dma("sp", cosT, cos_d)
        P.dma("sp", sinT, sin_d)
        ones_bf = P.sb([128, 128], BF16, "ones")
        P.memset("dve", ones_bf, 1.0)
        bandm = P.sb([128, 2, 128], BF16, "bandm")
        P.copy("dve", bandm[:, 0, :], m_gt)
        P.copy("dve", bandm[:, 1, :], m_le)
        epsb = P.sb([128, 1], F32, "epsb")
        P.memset("dve", epsb, EPS)
        onesb = P.sb([128, 1], F32, "onesb")
        P.memset("dve", onesb, 1.0)
        gains = P.sb([128, NL, 4, HD], F32, "gains")
        P.dma("sp", gains, gains_d)
        for l in (range(NL) if 'g' not in SKIP else []):
            P.ts("dve", gains[:, l, 0, :], gains[:, l, 0, :], 0.125, ALU.mult)
            P.ts("dve", gains[:, l, 2, :], gains[:, l, 2, :], 0.125, ALU.mult)
        esink = P.sb([128, NL, 4], F32, "esink")
        P.dma("sp", esink, sinks_d)
        if 'e' not in SKIP: P.act(esink, esink, AF.Exp)
        gout = P.sb([128, NL, 8], F32, "gout")
        P.dma("sp", gout, gout_d)
        ln1 = P.sb([128, NL, 8], F32, "ln1")
        ln2 = P.sb([128, NL, 8], F32, "ln2")
        P.dma("sp", ln1, ln1_d)
        P.dma("sp", ln2, ln2_d)
        bmod = P.sb([128, NL, 48], F32, "bmod")
        P.dma("sp", bmod, bmod_d)
        cact = P.sb([128, 8, NBC], F32, "cact")
        P.dma("sp", cact, cT_d)
        if 's' not in SKIP: P.act(cact, cact, AF.Silu)
        mod = P.sb([128, NL, 48, NBC], F32, "mod")
        A1 = P.sb([128, NL, 8, NBC], F32, "A1")
        A2 = P.sb([128, NL, 8, NBC], F32, "A2")

        PS = [P.ps(f"ps{i}") for i in range(8)]
        rr = {}

        def psum(pool):
            k = rr.get(pool, 0)
            rr[pool] = (k + 1) % len(pool)
            return PS[pool[k]]

        xT = [[P.sb([128, 512], F32, f"x{c}_{t}") for t in range(NT)] for c in range(8)]
        rstd = P.sb([128, 512], F32, "rstd")
        hT = [P.sb([128, 512], BF16, f"h{c}") for c in range(8)]
        sqt = [P.sb([128, 512], BF16, f"sq{i}") for i in range(2)]
        tmpf = [P.sb([128, 512], F32, f"tmpf{i}") for i in range(2)]

        ARENA = 113600
        P.arena_init(ARENA)

        wm = [P.ar([128, 8, 128], F32, f"wm{i}") for i in range(2)]
        it = 0
        for l in (layers if not os.environ.get('KNOMOD') else []):
            for j in range(48):
                w = wm[it % 2]
                it += 1
                P.dma("sp", w, V(wmod_d.ap[l, :, j * 128:(j + 1) * 128].rearrange("(k p) c -> p k c", p=128), wmod_d.buf))
                pm = psum((0, 1))
                for k in range(8):
                    P.mm(pm[:, 0:NBC], w[:, k, :], cact[:, k, :], start=(k == 0), stop=(k == 7))
                P.ts("dve", mod[:, l, j, :], pm[:, 0:NBC], bmod[:, l, j:j + 1], ALU.add)
        for l in (layers if 'a' not in SKIP else []):
            for c in range(8):
                P.ts("dve", A1[:, l, c, :], mod[:, l, 8 + c, :], 1.0, ALU.add, ln1[:, l, c:c + 1], ALU.mult)
                P.ts("dve", A2[:, l, c, :], mod[:, l, 32 + c, :], 1.0, ALU.add, ln2[:, l, c:c + 1], ALU.mult)
        P.barrier()

        def B1(l, c, b): return mod[:, l, 0 + c, b:b + 1]
        def G1(l, c, b): return mod[:, l, 16 + c, b:b + 1]
        def B2(l, c, b): return mod[:, l, 24 + c, b:b + 1]
        def G2(l, c, b): return mod[:, l, 40 + c, b:b + 1]

        P.arena_reset()
        kT = [P.ar([128, 2, 512], BF16, f"kT{t}") for t in range(NT)]
        kbT = [[P.ar([128, 512], BF16, f"kb{p}_{t}") for t in range(NT)] for p in range(2)]
        vall = [P.ar([128, 4, 448], BF16, f"v{t}") for t in range(NT)]
        qT = P.ar([128, 8, 512], BF16, "qT")
        qbT = [P.ar([128, 512], BF16, f"qb{p}") for p in range(2)]
        nqbT = [P.ar([128, 512], BF16, f"nqb{p}") for p in range(2)]
        wi = P.ar([128, 4, 4], F32, "wi")
        u_sq, u_t1 = tmpf
        u_y = P.ar([128, 512], F32, "u_y")
        u_r = P.ar([128, 512], F32, "u_r")
        u_o = P.ar([128, 512], BF16, "u_o")
        st_ss = P.ar([128, 8], F32, "st_ss")
        st_rs = P.ar([128, 8], F32, "st_rs")
        wA = P.ar([128, 8, 512], BF16, "wA")
        wB = P.ar([128, 8, 260], BF16, "wB")
        wo_s = [P.ar([128, 8, 128], BF16, f"wo{i}") for i in range(2)]
        sc = P.ar([128, L], F32, "sc")
        rel = [P.ar([128, 512], BF16, "rel0")] * 2
        msk = P.ar([128, L], BF16, "msk")
        junk = P.ar([128, L], U8, "junk")
        mT = P.ar([128, NB, 512], BF16, "mT")
        bs = {k: P.ar([128, 1], F32, "bs_" + k) for k in ("lo", "w0", "mid", "cnt", "t", "hi")}
        pT = [P.ar([128, 512], BF16, f"pT{i}") for i in range(3)]
        spb = [P.ar([128, 512], BF16, f"sp{i}") for i in range(2)]
        Rb = P.ar([128, 512], BF16, "Rb")
        on = [u_y, u_r]
        rden = P.ar([128, 512], F32, "rden")
        ef = rden
        oT = [P.ar([128, 512], BF16, f"o{c}") for c in range(8)]
        att_bytes = P.arena_off
        P.arena_reset()
        actT = [P.ar([128, 512], BF16, f"a{f}") for f in range(NFF)]
        sg = [P.ar([128, 512], F32, f"sg{i}") for i in range(2)]
        wgu = [P.ar([128, 8, 2, 256], BF16, f"wgu{i}") for i in range(2)]
        wdn = [P.ar([128, 2, 1024], BF16, f"wdn{i}") for i in range(2)]
        xstage = P.ar([128, 1024], F32, "xstage")

        def wslice(wb, l, cols0, ncols):
            return V(wb.ap[l, :, cols0:cols0 + ncols].rearrange("(k p) c -> p k c", p=128), wb.buf)

        def norm_stats(t):
            pm = psum((0, 1))
            for c in range(8):
                s = sqt[c % 2]
                P.act(s, xT[c][t], AF.Square)
                P.mm(pm, ones_bf, s, start=(c == 0), stop=(c == 7))
            P.act(tmpf[0], pm, AF.Sqrt, scale=1.0 / D, bias=epsb)
            P.recip(rstd, tmpf[0])

        def make_h(t, A, Bf, l, b):
            norm_stats(t)
            for c in range(8):
                tm = tmpf[c % 2]
                P.tt("dve", tm, xT[c][t], rstd, ALU.mult)
                P.act(hT[c], tm, AF.Identity, scale=A[:, l, c, b:b + 1], bias=Bf(l, c, b))

        def hview(v, w, perm):
            nh = w // 64
            if perm:
                return V(v.ap[:, :w].rearrange("p (i s two d) -> p s i two d", s=2, two=2, d=32), v.buf)
            return V(v.ap[:, :w].rearrange("p (s i two d) -> p s i two d", s=2, two=2, d=32), v.buf)

        def rope_norm(pu, nh, tb_glob, gsegs, perm):
            w = nh * 64
            if any(g is not None for (_, _, g) in gsegs):
                P.act(u_sq[:, :w], pu[:, :w], AF.Square)
                P.reduce(st_ss[:, :nh], V(u_sq.ap[:, :w].rearrange("p (h d) -> p h d", d=64), u_sq.buf), ALU.add)
                P.act(st_rs[:, :nh], st_ss[:, :nh], AF.Sqrt, scale=1.0 / HD, bias=epsb)
                P.recip(st_rs[:, :nh], st_rs[:, :nh])
            for (h0, h1, g) in gsegs:
                n = h1 - h0
                src = V(pu.ap[:, h0 * 64:h1 * 64].rearrange("p (h d) -> p h d", d=64), pu.buf)
                dst = V(u_y.ap[:, h0 * 64:h1 * 64].rearrange("p (h d) -> p h d", d=64), u_y.buf)
                if g is None:
                    P.copy("act", dst, src)
                else:
                    P.tt("dve", dst, src, V(st_rs.ap[:, h0:h1].unsqueeze(2).to_broadcast([128, n, 64]), st_rs.buf), ALU.mult)
                    P.tt("pool", dst, dst, V(g.ap.unsqueeze(1).to_broadcast([128, n, 64]), g.buf), ALU.mult)
            y5 = hview(u_y, w, False)
            t5 = hview(u_t1, w, False)
            r5 = hview(u_r, w, False)
            o5 = hview(u_o, w, perm)
            hh = nh // 2
            cb = V(cosT.ap[:, tb_glob, :].unsqueeze(1).unsqueeze(1).unsqueeze(1).to_broadcast([128, 2, hh, 2, 32]), cosT.buf)
            sb_ = V(sinT.ap[:, tb_glob, :].unsqueeze(1).unsqueeze(1).to_broadcast([128, 2, hh, 32]), sinT.buf)
            for s_ in range(2):
                P.tt("dve", t5[:, s_], y5[:, s_], cb[:, s_], ALU.mult)
            P.tt("pool", r5[:, :, :, 0, :], y5[:, :, :, 1, :], sb_, ALU.mult)
            P.tt("pool", r5[:, :, :, 1, :], y5[:, :, :, 0, :], sb_, ALU.mult)
            P.tt("dve", o5[:, :, :, 0, :], t5[:, :, :, 0, :], r5[:, :, :, 0, :], ALU.subtract)
            P.tt("dve", o5[:, :, :, 1, :], t5[:, :, :, 1, :], r5[:, :, :, 1, :], ALU.add)

        def transpose_to(dst3, nchunks):
            ptr = psum((6, 7))
            ptb = V(ptr.ap.bitcast(BF16), ptr.buf)
            for i in range(nchunks):
                P.tr(ptb[:, i * 128:(i + 1) * 128], u_o[:, i * 128:(i + 1) * 128], ident)
            P.copy("act", dst3, V(ptb.ap[:, :nchunks * 128].rearrange("p (i q) -> p i q", q=128), ptb.buf))

        def stage_kv(l, b):
            P.dma("sp", wB[:, :, 0:256], wslice(win_b, l, O_KB, 256))
            for t in range(NT):
                make_h(t, A1, B1, l, b)
                for p in range(2):
                    pm = psum((0, 1))
                    for k in range(8):
                        P.mm(pm, wB[:, k, p * 128:(p + 1) * 128], hT[k], start=(k == 0), stop=(k == 7))
                    P.copy("act", kbT[p][t], pm)
                o = 0
                for (c0, n) in ((O_KA, 64), (O_KI, 64), (O_KC, 128)):
                    P.dma("sp", wA[:, :, o:o + n], wslice(win_b, l, c0, n))
                    o += n
                for tb in range(4):
                    pk = psum((2, 3))
                    for k in range(8):
                        P.mm(pk[:, :256], hT[k][:, tb * 128:(tb + 1) * 128], wA[:, k, 0:256], start=(k == 0), stop=(k == 7))
                    rope_norm(pk, 4, t * 4 + tb, [(0, 1, gains[:, l, 1, :]), (1, 2, None), (2, 4, gains[:, l, 3, :])], False)
                    transpose_to(kT[t][:, :, tb * 128:(tb + 1) * 128], 2)
                o = 0
                for (c0, n) in ((O_VA, 64), (O_VB, 256), (O_VC, 128)):
                    P.dma("sp", wA[:, :, o:o + n], wslice(win_b, l, c0, n))
                    o += n
                for tb in range(4):
                    pv = psum((4, 5))
                    for k in range(8):
                        P.mm(pv[:, :448], hT[k][:, tb * 128:(tb + 1) * 128], wA[:, k, 0:448], start=(k == 0), stop=(k == 7))
                    P.copy("act", vall[t][:, tb, :], pv[:, :448])

        def stage_q(l, b, G):
            make_h(G, A1, B1, l, b)
            P.dma("sp", wB[:, :, 0:256], wslice(win_b, l, O_QB, 256))
            P.dma("sp", wB[:, :, 256:260], wslice(win_b, l, O_WI, 4))
            for p in range(2):
                pm = psum((0, 1))
                for k in range(8):
                    P.mm(pm, wB[:, k, p * 128:(p + 1) * 128], hT[k], start=(k == 0), stop=(k == 7))
                P.act(qbT[p], pm, AF.Copy, scale=0.125)
                P.act(nqbT[p], pm, AF.Copy, scale=-0.125)
            for tb in range(4):
                pw = psum((0, 1))
                for k in range(8):
                    P.mm(pw[:, 0:4], hT[k][:, tb * 128:(tb + 1) * 128], wB[:, k, 256:260], start=(k == 0), stop=(k == 7))
                P.act(wi[:, tb, :], pw[:, 0:4], AF.Copy, scale=1.0 / 16.0)
            P.dma("sp", wA[:, :, 0:256], wslice(win_b, l, O_QA, 256))
            P.dma("sp", wA[:, :, 256:512], wslice(win_b, l, O_QI, 256))
            for tb in range(4):
                pa = psum((2, 3))
                for k in range(8):
                    P.mm(pa, hT[k][:, tb * 128:(tb + 1) * 128], wA[:, k, :], start=(k == 0), stop=(k == 7))
                rope_norm(pa, 8, G * 4 + tb, [(0, 4, gains[:, l, 0, :]), (4, 8, None)], True)
                transpose_to(qT[:, 0:4, tb * 128:(tb + 1) * 128], 4)
            P.dma("sp", wA, wslice(win_b, l, O_QC, 512))
            for tb in range(4):
                pc = psum((4, 5))
                for k in range(8):
                    P.mm(pc, hT[k][:, tb * 128:(tb + 1) * 128], wA[:, k, :], start=(k == 0), stop=(k == 7))
                rope_norm(pc, 8, G * 4 + tb, [(0, 8, gains[:, l, 2, :])], True)
                transpose_to(qT[:, 4:8, tb * 128:(tb + 1) * 128], 4)

        def keyblk(j):
            return j // 4, (j % 4) * 128

        LO = slice(0, 64)
        HI = slice(64, 128)

        def topk_block(G, nn):
            n0 = 4 * G
            n = n0 + nn
            K = (n + 1) * 128
            qsl = slice(nn * 128, (nn + 1) * 128)
            if K <= NSEL:
                for j in range(n + 1):
                    P.copy("pool", mT[:, j, qsl], ones_bf if j < n else m_le)
                return
            for kc in range((K + 511) // 512):
                k0 = kc * 512
                kw = min(512, K - k0)
                for h in range(4):
                    pm = PS[2 + h % 2]
                    P.mm(pm[:, :kw], qT[HI, h, qsl], kT[kc][HI, 0, 0:kw])
                    r = rel[h % 2]
                    P.act(r[:, :kw], pm[:, :kw], AF.Relu)
                    if h == 0:
                        P.ts("dve", sc[:, k0:k0 + kw], r[:, :kw], wi[:, nn, 0:1], ALU.mult)
                    else:
                        P.stt(sc[:, k0:k0 + kw], r[:, :kw], wi[:, nn, h:h + 1], sc[:, k0:k0 + kw], ALU.mult, ALU.add)
            P.reduce(bs["hi"], sc[:, :K], ALU.max)
            P.reduce(bs["lo"], sc[:, :K], ALU.min)
            P.tt("dve", bs["w0"], bs["hi"], bs["lo"], ALU.subtract)
            P.tt("dve", sc[:, n * 128:K], sc[:, n * 128:K], negm, ALU.add)
            for i in range(1, NIT + 1):
                f = 2.0 ** (-i)
                P.stt(bs["mid"], bs["w0"], f, bs["lo"], ALU.mult, ALU.add)
                P.ts("dve", junk[:, :K], sc[:, :K], bs["mid"], ALU.is_ge, 0.0, ALU.add, accum=bs["cnt"])
                P.ts("dve", bs["t"], bs["cnt"], float(NSEL), ALU.is_ge, f, ALU.mult)
                P.stt(bs["lo"], bs["t"], bs["w0"], bs["lo"], ALU.mult, ALU.add)
            P.ts("dve", msk[:, :K], sc[:, :K], bs["lo"], ALU.is_ge)

        def mask_transposes(G, nn):
            n = 4 * G + nn
            K = (n + 1) * 128
            qsl = slice(nn * 128, (nn + 1) * 128)
            if K <= NSEL:
                return
            for j0 in range(0, n + 1, 8):
                j1 = min(n + 1, j0 + 8)
                ptr = psum((6, 7))
                ptb = V(ptr.ap.bitcast(BF16), ptr.buf)
                for j in range(j0, j1):
                    P.tr(ptb[:, (j - j0) * 128:(j - j0 + 1) * 128], msk[:, j * 128:(j + 1) * 128], ident)
                P.copy("act", mT[:, j0:j1, qsl],
                       V(ptb.ap[:, :(j1 - j0) * 128].rearrange("p (i q) -> p i q", q=128), ptb.buf))

        def norm_heads(l, chunks):
            for i, c in enumerate(chunks):
                s = sqt[i % 2]
                P.act(s, on[i], AF.Square)
                pm = psum((6, 7))
                P.mm(pm, bones, s)
                P.act(rden, pm, AF.Sqrt, scale=1.0 / HD, bias=epsb)
                P.recip(rden, rden)
                P.stt(oT[c], on[i], gout[:, l, c:c + 1], rden, ALU.mult, ALU.mult)

        def dsa(G, l):
            n0 = 4 * G
            jmax = n0 + 3
            po = [PS[0], PS[1]]
            pd = [PS[2], PS[3]]
            for j in range(jmax + 1):
                qs = max(0, j - n0) * 128
                N = 512 - qs
                tk, ko = keyblk(j)
                for h in range(4):
                    pr, half = h // 2, h % 2
                    hs = HI if half else LO
                    pz = psum((4, 5))
                    P.mm(pz[:, :N], kT[tk][LO, 0, ko:ko + 128], qT[LO, h, qs:512])
                    pt = pT[(j * 4 + h) % 3]
                    P.act(pt[:, :N], pz[:, :N], AF.Exp)
                    P.tt("pool", pt[:, :N], pt[:, :N], mT[:, j, qs:512], ALU.mult)
                    P.mm(po[pr][hs, qs:512], vall[tk][:, j % 4, 0:64], pt[:, :N], start=(j == 0), stop=(j == jmax))
                    P.mm(pd[pr][hs, qs:512], ones_bf[:, 0:64], pt[:, :N], start=(j == 0), stop=(j == jmax))
            for pr in range(2):
                P.recip(rden, pd[pr])
                P.tt("dve", on[pr], po[pr], rden, ALU.mult)
            norm_heads(l, [0, 1])

        def sb_head(G, h):
            n0 = 4 * G
            jmax = n0 + 3
            po = [PS[0], PS[1]]
            pr, half = h // 2, h % 2
            hs = HI if half else LO
            P.memset("pool", Rb, 0.0)
            for j in range(jmax, -1, -1):
                qs = max(0, j - n0) * 128
                N = 512 - qs
                tk, ko = keyblk(j)
                diag = j >= n0
                pz = psum((4, 5))
                P.mm(pz[:, :N], kbT[pr][tk][hs, ko:ko + 128], qbT[pr][hs, qs:512])
                s = spb[j % 2]
                P.act(ef[:, :N], pz[:, :N], AF.Exp)
                P.act(s[:, :N], ef[:, :N], AF.Ln, bias=onesb)
                if diag:
                    P.tt("pool", s[:, 0:128], s[:, 0:128], m_lt, ALU.mult)
                pc = psum((6, 7))
                P.mm(pc[:, :N], kbT[pr][tk][hs, ko:ko + 128], nqbT[pr][hs, qs:512], start=True, stop=False)
                P.mm(pc[:, :N], tinc, s[:, :N], start=False, stop=(j == jmax))
                if j < jmax:
                    P.mm(pc[:, :N], ones_bf, Rb[:, qs:512], start=False, stop=True)
                a = pT[j % 3]
                P.act(a[:, :N], pc[:, :N], AF.Exp, scale=-1.0)
                if diag:
                    P.tt("pool", a[:, 0:128], a[:, 0:128], m_lt, ALU.mult)
                if j > 0:
                    P.tt("pool", Rb[:, qs:512], Rb[:, qs:512], s[:, :N], ALU.add)
                P.mm(po[pr][hs, qs:512], vall[tk][:, j % 4, 64 + h * 64:64 + (h + 1) * 64], a[:, :N],
                     start=(j == jmax), stop=(j == 0))

        def sb_finish(l):
            for pr in range(2):
                P.copy("dve", on[pr], PS[pr])
            norm_heads(l, [2, 3])

        def swa(G, l):
            n0 = 4 * G
            for g in range(2):
                gs = HI if g else LO
                po = [PS[0], PS[1]]
                pd = [PS[2], PS[3]]
                for nn in range(4):
                    n = n0 + nn
                    kbs = [n - 1, n] if n > 0 else [n]
                    pz = [PS[4], PS[5]]
                    pts = {}
                    for kb in kbs:
                        i = kb - n + 1
                        tk, ko = keyblk(kb)
                        P.mm(V(pz[i].ap.rearrange("p (c q) -> p c q", q=128), pz[i].buf),
                             kT[tk][gs, 1, ko:ko + 128], qT[gs, 4:8, nn * 128:(nn + 1) * 128])
                        pt = pT[(nn * 2 + i) % 3]
                        pts[kb] = pt
                        P.act(pt, pz[i], AF.Exp)
                        P.tt("pool", V(pt.ap.rearrange("p (a q) -> p a q", q=128), pt.buf),
                             V(pt.ap.rearrange("p (a q) -> p a q", q=128), pt.buf),
                             V(bandm.ap[:, i, :].unsqueeze(1).to_broadcast([128, 4, 128]), bandm.buf), ALU.mult)
                    bank = nn // 2
                    off = (nn % 2) * 256
                    for half in range(2):
                        hs = HI if half else LO
                        for kb in kbs:
                            tk, ko = keyblk(kb)
                            rhs = V(pts[kb].ap.rearrange("p (c s q) -> p s c q", s=2, q=128)[:, half], pts[kb].buf)
                            outv = V(po[bank].ap[hs, off:off + 256].rearrange("p (c q) -> p c q", q=128), po[bank].buf)
                            outd = V(pd[bank].ap[hs, off:off + 256].rearrange("p (c q) -> p c q", q=128), pd[bank].buf)
                            P.mm(outv, vall[tk][:, kb % 4, 320 + g * 64:320 + (g + 1) * 64], rhs,
                                 start=(kb == kbs[0]), stop=(kb == kbs[-1]))
                            P.mm(outd, ones_bf[:, 0:64], rhs, start=(kb == kbs[0]), stop=(kb == kbs[-1]))
                for cc in range(2):
                    ch = 2 * g + cc
                    for bank in range(2):
                        den = V(pd[bank].ap.rearrange("p (n c q) -> p n c q", n=2, c=2)[:, :, cc, :], pd[bank].buf)
                        num = V(po[bank].ap.rearrange("p (n c q) -> p n c q", n=2, c=2)[:, :, cc, :], po[bank].buf)
                        rd = V(rden.ap[:, 0:256].rearrange("p (n q) -> p n q", q=128), rden.buf)
                        P.ts("dve", rd, den, esink[:, l, ch:ch + 1], ALU.add)
                        P.recip(rd, rd)
                        dst = V(on[cc].ap[:, bank * 256:(bank + 1) * 256].rearrange("p (n q) -> p n q", q=128), on[cc].buf)
                        P.tt("dve", dst, num, rd, ALU.mult)
                norm_heads(l, [4 + 2 * g, 5 + 2 * g])

        def out_proj(l, b, G):
            for co in range(8):
                w = wo_s[co % 2]
                P.dma("sp", w, wslice(wo_b, l, co * 128, 128))
                pm = psum((4, 5))
                for k in range(8):
                    P.mm(pm, w[:, k, :], oT[k], start=(k == 0), stop=(k == 7))
                P.stt(xT[co][G], pm, G1(l, co, b), xT[co][G], ALU.mult, ALU.add)

        def ffn(l, b):
            for t in range(NT):
                make_h(t, A2, B2, l, b)
                for f2 in range(NFF // 2):
                    w = wgu[f2 % 2]
                    P.dma("sp", w[:, :, 0, :], wslice(wg_b, l, f2 * 256, 256))
                    P.dma("sp", w[:, :, 1, :], wslice(wu_b, l, f2 * 256, 256))
                    for ff in range(2):
                        f = f2 * 2 + ff
                        pg = psum((0, 1))
                        pu = psum((2, 3))
                        for k in range(8):
                            P.mm(pg, w[:, k, 0, ff * 128:(ff + 1) * 128], hT[k], start=(k == 0), stop=(k == 7))
                        for k in range(8):
                            P.mm(pu, w[:, k, 1, ff * 128:(ff + 1) * 128], hT[k], start=(k == 0), stop=(k == 7))
                        P.act(sg[f % 2], pg, AF.Silu)
                        P.tt("dve", actT[f], sg[f % 2], pu, ALU.mult)
                pacc = [PS[4], PS[5], PS[6], PS[7]]
                for half in range(2):
                    for f2 in range(NFF // 2):
                        w = wdn[f2 % 2]
                        P.dma("sp", w, V(wd_b.ap[l, f2 * 256:(f2 + 1) * 256, :].rearrange("(k p) c -> p k c", p=128), wd_b.buf))
                        for ff in range(2):
                            f = f2 * 2 + ff
                            for c4 in range(4):
                                co = half * 4 + c4
                                P.mm(pacc[c4], w[:, ff, co * 128:(co + 1) * 128], actT[f], start=(f == 0), stop=(f == NFF - 1))
                    for c4 in range(4):
                        co = half * 4 + c4
                        P.stt(xT[co][t], pacc[c4], G2(l, co, b), xT[co][t], ALU.mult, ALU.add)

        for b in range(NBC):
            P.barrier()
            for t in (range(NT) if 'x' not in SKIP else []):
                for tb in range(4):
                    r0 = (t * 4 + tb) * 128
                    P.dma("sp", xstage, V(x_d.ap[b, r0:r0 + 128, :], x_d.buf))
                    for c4 in range(0, 8, 4):
                        pm = psum((0, 1, 2, 3))
                        for c in range(c4, c4 + 4):
                            P.tr(pm[:, (c - c4) * 128:(c - c4 + 1) * 128], xstage[:, c * 128:(c + 1) * 128], ident_f)
                        for c in range(c4, c4 + 4):
                            P.copy("act" if c % 2 else "dve", xT[c][t][:, tb * 128:(tb + 1) * 128],
                                   pm[:, (c - c4) * 128:(c - c4 + 1) * 128])
            for l in layers:
                P.barrier()
                if STOP < 1: continue
                stage_kv(l, b)
                for G in range(NT):
                    if STOP < 2: continue
                    stage_q(l, b, G)
                    if STOP < 3: continue
                    for nn in range(4):
                        topk_block(G, nn)
                        sb_head(G, nn)
                        mask_transposes(G, nn)
                    sb_finish(l)
                    swa(G, l)
                    dsa(G, l)
                    out_proj(l, b, G)
                P.barrier()
                if STOP < 8: continue
                ffn(l, b)
            P.barrier()
            for t in range(NT):
                for tb in range(4):
                    r0 = (t * 4 + tb) * 128
                    for c4 in (range(0, 8, 4) if 'y' not in SKIP else []):
                        pm = psum((0, 1, 2, 3))
                        for c in range(c4, c4 + 4):
                            P.tr(pm[:, (c - c4) * 128:(c - c4 + 1) * 128], xT[c][t][:, tb * 128:(tb + 1) * 128], ident_f)
                        P.copy("act" if c4 else "dve", xstage[:, c4 * 128:(c4 + 4) * 128], pm)
                    P.dma("sp", V(y_d.ap[b, r0:r0 + 128, :], y_d.buf), xstage, out_is_ext=True)

        print("arena bytes: att", att_bytes, "max", P.arena_max, "ops", {k: len(v) for k, v in P.ops.items()})
        block = es.enter_context(nc.Block())
        P.emit(block)
    return nc


_CACHE = {}


def _host_consts(L):
    NB = L // 128
    kk = np.arange(128)[:, None]
    qq = np.arange(128)[None, :]
    cst = np.zeros((128, 6, 128), np.float32)
    cst[:, 0] = np.eye(128)
    cst[:, 1] = (kk <= qq)
    cst[:, 2] = (kk < qq)
    cst[:, 3] = (kk > qq)
    cst[:, 4] = (kk >= qq)
    cst[:, 5] = ((kk // 64) == (qq // 64))
    negm = np.where(qq <= kk, 0.0, NEG).astype(np.float32)
    inv = (1.0 / (np.float32(10000.0) ** (np.arange(0, 64, 2, dtype=np.float32) / np.float32(64)))).astype(np.float32)
    ang = np.arange(L, dtype=np.float32)[:, None] * inv[None, :]
    cos = np.cos(ang).astype(np.float32).reshape(NB, 128, 32).transpose(1, 0, 2)
    sin = np.sin(ang).astype(np.float32).reshape(NB, 128, 32).transpose(1, 0, 2)
    return cst, negm, np.ascontiguousarray(cos), np.ascontiguousarray(sin)


def _run(inputs, L, NBC, ncores, layers):
    key = (L, NBC, tuple(layers))
    if key not in _CACHE:
        _CACHE[key] = build_program(L, NBC, layers)
    nc = _CACHE[key]
    f = lambda a: np.ascontiguousarray(np.asarray(a, dtype=np.float32))
    cst, negm, cos, sin = _host_consts(L)
    x = f(inputs["x"])
    c = f(inputs["c"])
    NL = 4
    def featT(a):
        a = f(a)
        return np.ascontiguousarray(a.reshape(NL, -1, 128).transpose(2, 0, 1))
    gains = np.stack([f(inputs["qn_a"]), f(inputs["kn_a"]), f(inputs["qn_c"]), f(inputs["kn_c"])], axis=1)
    gainsB = np.ascontiguousarray(np.broadcast_to(gains[None], (128, NL, 4, 64)))
    sk = f(inputs["sinks"])
    sinksT = np.zeros((128, NL, 4), np.float32)
    for ch in range(4):
        sinksT[:64, :, ch] = sk[:, ch][None, :]
        sinksT[64:, :, ch] = sk[:, 4 + ch][None, :]
    common = {
        "ln1T": featT(inputs["ln1"]), "ln2T": featT(inputs["ln2"]), "bmodT": featT(inputs["b_mod"]),
        "w_mod": f(inputs["w_mod"]), "w_in": f(inputs["w_in"]), "w_o": f(inputs["w_o"]),
        "w_gate": f(inputs["w_gate"]), "w_up": f(inputs["w_up"]), "w_down": f(inputs["w_down"]),
        "gainsB": gainsB, "sinksT": sinksT, "goutT": featT(f(inputs["g_out"]).reshape(NL, -1)),
        "cosT": cos, "sinT": sin, "consts": cst, "negm": negm,
    }
    in_maps = []
    for i in range(ncores):
        cb = c[i * NBC:(i + 1) * NBC]
        cT = np.ascontiguousarray(cb.reshape(NBC, 8, 128).transpose(2, 1, 0))
        m = dict(common)
        m["x"] = np.ascontiguousarray(x[i * NBC:(i + 1) * NBC])
        m["cT"] = cT
        in_maps.append(m)
    res = run_bass_kernel_spmd(nc, in_maps, core_ids=list(range(ncores)))
    return np.concatenate([r["y"] for r in res.results], axis=0)


def kernel(x, c, ln1, ln2, w_mod, b_mod, w_in, qn_a, kn_a, qn_c, kn_c, sinks, g_out, w_o, w_gate, w_up, w_down):
    inputs = dict(x=x, c=c, ln1=ln1, ln2=ln2, w_mod=w_mod, b_mod=b_mod, w_in=w_in, qn_a=qn_a, kn_a=kn_a,
                  qn_c=qn_c, kn_c=kn_c, sinks=sinks, g_out=g_out, w_o=w_o, w_gate=w_gate, w_up=w_up, w_down=w_down)
    B, L = x.shape[0], x.shape[1]
    out = _run(inputs, L, B // 8, 8, [0, 1, 2, 3])
    return out.astype(np.float32)
```

```python
import contextlib
import numpy as np
import concourse.bass as bass
import concourse.mybir as mybir
from concourse.bass_utils import run_bass_kernel_spmd

F32 = mybir.dt.float32
BF16 = mybir.dt.bfloat16
U32 = mybir.dt.uint32
FP8 = mybir.dt.float8e4
U8 = mybir.dt.uint8
AF = mybir.ActivationFunctionType
ALU = mybir.AluOpType
AX = mybir.AxisListType

D = 1024
DFF = 2816
NFF = DFF // 128
INC = 2244
HD = 64
EPS = 1e-6
NEG = -1.0e30
NIT = 17
import os
STOP = int(os.environ.get('KSTOP', '9'))
SKIP = os.environ.get('KSKIP', '')
O_QA, O_KA, O_VA, O_QI, O_KI, O_WI, O_QB, O_KB, O_VB, O_QC, O_KC, O_VC = (
    0, 256, 320, 384, 640, 704, 708, 964, 1220, 1476, 1988, 2116)


class Buf:
    __slots__ = ("name", "w", "r", "dsem", "excl")

    def __init__(self, name, excl=False):
        self.name = name
        self.excl = excl
        self.w = None
        self.r = []
        self.dsem = None


class V:
    __slots__ = ("ap", "buf")

    def __init__(self, ap, buf):
        self.ap = ap
        self.buf = buf

    def __getitem__(self, idx):
        return V(self.ap[idx], self.buf)


class Prog:
    ENG = ("pe", "dve", "act", "pool", "sp")

    def __init__(self, nc, es):
        self.nc = nc
        self.es = es
        self.ops = {e: [] for e in self.ENG}
        self.cnt = {}
        self.seen = {e: {} for e in self.ENG}
        self.sems = {}
        for e in ("pe", "dve", "act", "pool"):
            self.sems[e] = es.enter_context(nc.semaphore("S_" + e))
            self.cnt[e] = 0
        self.ndsem = 0
        self.nt = 0
        self.out_waits = []

    def sb(self, shape, dt, name=None):
        self.nt += 1
        name = name or f"t{self.nt}"
        t = self.es.enter_context(self.nc.sbuf_tensor(name + f"_{self.nt}", list(shape), dt))
        return V(t[:], Buf(name))

    def ps(self, name):
        self.nt += 1
        t = self.es.enter_context(self.nc.psum_tensor(name + f"_{self.nt}", [128, 512], F32))
        return V(t[:], Buf(name, excl=True))

    def arena_init(self, nbytes):
        t = self.es.enter_context(self.nc.sbuf_tensor("arena", [128, nbytes // 4], F32))
        self.arena = t[:]
        self.arena_n = nbytes
        self.arena_off = 0
        self.arena_max = 0

    def arena_reset(self):
        self.arena_off = 0

    def ar(self, shape, dt, name):
        n = 1
        for d in shape[1:]:
            n *= d
        esz = {F32: 4, BF16: 2, U32: 4, FP8: 1, U8: 1}[dt]
        nb = (n * esz + 3) // 4 * 4
        off = self.arena_off
        self.arena_off += nb
        self.arena_max = max(self.arena_max, self.arena_off)
        assert self.arena_off <= self.arena_n, (name, self.arena_off, self.arena_n)
        ap = self.arena[:, off // 4:(off + nb) // 4]
        if dt != F32:
            ap = ap.bitcast(dt)
        ap = ap[:, 0:n]
        if len(shape) == 3:
            ap = ap.rearrange("p (a b) -> p a b", b=shape[2])
        elif len(shape) == 4:
            ap = ap.rearrange("p (a b c) -> p a b c", b=shape[2], c=shape[3])
        return V(ap, Buf(name))

    def dram(self, name, shape, dt):
        t = self.nc.dram_tensor(name, list(shape), dt, kind="Internal")
        return V(t.ap(), Buf(name))

    def dsem(self, buf):
        if buf.dsem is None:
            key = f"d{self.ndsem}"
            self.ndsem += 1
            self.sems[key] = self.es.enter_context(self.nc.semaphore("D_" + key))
            self.cnt[key] = 0
            buf.dsem = key
        return buf.dsem

    def _deps(self, eng, reads, writes):
        need = {}
        for b in reads:
            if b.w is not None:
                k, v = b.w
                need[k] = max(need.get(k, 0), v)
            if b.excl:
                for (k, v) in b.r:
                    if k != eng:
                        need[k] = max(need.get(k, 0), v)
        for b in writes:
            if b.w is not None:
                k, v = b.w
                need[k] = max(need.get(k, 0), v)
            for (k, v) in b.r:
                need[k] = max(need.get(k, 0), v)
        waits = []
        seen = self.seen[eng]
        for k, v in need.items():
            if k == eng:
                if eng == "pe":
                    continue
            if seen.get(k, 0) >= v:
                continue
            seen[k] = v
            waits.append((k, v))
        return waits

    def _mark(self, key, val, reads, writes):
        for b in reads:
            b.r.append((key, val))
            if len(b.r) > 24:
                m = {}
                for (k, v) in b.r:
                    m[k] = max(m.get(k, 0), v)
                b.r = list(m.items())
        for b in writes:
            b.w = (key, val)
            b.r = []

    def op(self, eng, fn, reads, writes):
        rb = [x.buf for x in reads]
        wb = [x.buf for x in writes]
        waits = self._deps(eng, rb, wb)
        self.cnt[eng] += 1
        self._mark(eng, self.cnt[eng], rb, wb)
        self.ops[eng].append((fn, waits, (eng, 1)))

    def dma(self, q, out, in_, out_is_ext=False):
        rb = [in_.buf]
        wb = [out.buf]
        waits = self._deps(q, rb, wb)
        key = self.dsem(out.buf)
        self.cnt[key] += 16
        self._mark(key, self.cnt[key], rb, wb)
        oa, ia = out.ap, in_.ap
        self.ops[q].append((lambda e: e.dma_start(out=oa, in_=ia), waits, (key, 16)))
        if out_is_ext:
            self.out_waits.append((key, self.cnt[key]))

    def barrier(self):
        for e in self.ENG:
            waits = []
            for k, v in self.cnt.items():
                if k == e or v == 0:
                    continue
                if self.seen[e].get(k, 0) >= v:
                    continue
                self.seen[e][k] = v
                waits.append((k, v))
            if waits:
                self.ops[e].append((None, waits, None))

    def emit(self, block):
        nc = self.nc
        engs = {"pe": (block.tensor, None), "dve": (block.vector, None), "act": (block.scalar, None),
                "pool": (block.gpsimd, None), "sp": (block.sync, None)}
        self.ops["sp"].append((None, list(self.out_waits), None))
        for name in self.ENG:
            ops = self.ops[name]
            sems = self.sems

            def body(e, ops=ops):
                for (fn, waits, inc) in ops:
                    for (k, v) in waits:
                        e.wait_ge(sems[k], v)
                    if fn is not None:
                        ins = fn(e)
                        ins.then_inc(sems[inc[0]], inc[1])
            engs[name][0](body)

    def mm(self, out, lhsT, rhs, start=True, stop=True):
        o, l, r = out.ap, lhsT.ap, rhs.ap
        self.op("pe", lambda e: e.matmul(o, lhsT=l, rhs=r, start=start, stop=stop, skip_group_check=True),
                [lhsT, rhs], [out])

    def tr(self, out, in_, ident):
        o, i, d = out.ap, in_.ap, ident.ap
        self.op("pe", lambda e: e.transpose(o, i, d), [in_, ident], [out])

    def act(self, out, in_, func, scale=1.0, bias=None, extra=()):
        o, i = out.ap, in_.ap
        rd = [in_] + list(extra)
        sc = scale.ap if isinstance(scale, V) else scale
        if isinstance(scale, V):
            rd.append(scale)
        if isinstance(bias, V):
            rd.append(bias)
            bi = bias.ap
            self.op("act", lambda e: e.activation(out=o, in_=i, func=func, bias=bi, scale=sc), rd, [out])
        elif bias is None:
            self.op("act", lambda e: e.activation(out=o, in_=i, func=func, scale=sc), rd, [out])
        else:
            self.op("act", lambda e: e.activation(out=o, in_=i, func=func, bias=bias, scale=sc), rd, [out])

    def ts(self, eng, out, in0, s1, op0, s2=None, op1=None, accum=None):
        o, i = out.ap, in0.ap
        rd = [in0]
        a1 = s1.ap if isinstance(s1, V) else s1
        a2 = s2.ap if isinstance(s2, V) else s2
        if isinstance(s1, V):
            rd.append(s1)
        if isinstance(s2, V):
            rd.append(s2)
        wr = [out]
        kw = {}
        if op1 is not None:
            kw["op1"] = op1
        if accum is not None:
            kw["accum_out"] = accum.ap
            wr.append(accum)
        self.op(eng, lambda e: e.tensor_scalar(out=o, in0=i, scalar1=a1, scalar2=a2, op0=op0, **kw), rd, wr)

    def tt(self, eng, out, in0, in1, op):
        o, a, b = out.ap, in0.ap, in1.ap
        self.op(eng, lambda e: e.tensor_tensor(out=o, in0=a, in1=b, op=op), [in0, in1], [out])

    def stt(self, out, in0, scalar, in1, op0, op1):
        o, a, b = out.ap, in0.ap, in1.ap
        rd = [in0, in1]
        s = scalar.ap if isinstance(scalar, V) else scalar
        if isinstance(scalar, V):
            rd.append(scalar)
        self.op("dve", lambda e: e.scalar_tensor_tensor(out=o, in0=a, scalar=s, in1=b, op0=op0, op1=op1), rd, [out])

    def copy(self, eng, out, in_):
        o, i = out.ap, in_.ap
        if eng == "act":
            self.op("act", lambda e: e.activation(out=o, in_=i, func=AF.Copy), [in_], [out])
        else:
            self.op(eng, lambda e: e.tensor_copy(out=o, in_=i), [in_], [out])

    def memset(self, eng, out, val):
        o = out.ap
        self.op(eng, lambda e: e.memset(o, val), [], [out])

    def recip(self, out, in_):
        o, i = out.ap, in_.ap
        self.op("dve", lambda e: e.reciprocal(out=o, in_=i), [in_], [out])

    def reduce(self, out, in_, op):
        o, i = out.ap, in_.ap
        self.op("dve", lambda e: e.tensor_reduce(out=o, in_=i, axis=AX.X, op=op), [in_], [out])


def build_program(L, NBC, layers):
    NB = L // 128
    NT = L // 512
    NSEL = min(256, L // 4)
    NL = 4
    nc = bass.Bass("TRN2", target_bir_lowering=False)
    es = contextlib.ExitStack()
    with es:
        P = Prog(nc, es)

        def ext(name, shape, dt=F32, out=False):
            t = nc.dram_tensor(name, list(shape), dt, kind="ExternalOutput" if out else "ExternalInput")
            return V(t.ap(), Buf(name))

        x_d = ext("x", [NBC, L, D])
        y_d = ext("y", [NBC, L, D], out=True)
        cT_d = ext("cT", [128, 8, NBC])
        ln1_d = ext("ln1T", [128, NL, 8])
        ln2_d = ext("ln2T", [128, NL, 8])
        bmod_d = ext("bmodT", [128, NL, 48])
        wmod_d = ext("w_mod", [NL, D, 6 * D])
        win_d = ext("w_in", [NL, D, INC])
        wo_d = ext("w_o", [NL, D, D])
        wg_d = ext("w_gate", [NL, D, DFF])
        wu_d = ext("w_up", [NL, D, DFF])
        wd_d = ext("w_down", [NL, DFF, D])
        gains_d = ext("gainsB", [128, NL, 4, HD])
        sinks_d = ext("sinksT", [128, NL, 4])
        gout_d = ext("goutT", [128, NL, 8])
        cos_d = ext("cosT", [128, NB, 32])
        sin_d = ext("sinT", [128, NB, 32])
        cst_d = ext("consts", [128, 6, 128])
        neg_d = ext("negm", [128, 128])

        win_b = P.dram("win_b", [NL, D, INC], BF16)
        wo_b = P.dram("wo_b", [NL, D, D], BF16)
        wg_b = P.dram("wg_b", [NL, D, DFF], BF16)
        wu_b = P.dram("wu_b", [NL, D, DFF], BF16)
        wd_b = P.dram("wd_b", [NL, DFF, D], BF16)
        for (dst, src, rows) in ((win_b, win_d, D), (wo_b, wo_d, D), (wg_b, wg_d, D), (wu_b, wu_d, D), (wd_b, wd_d, DFF)):
            for l in (layers if not os.environ.get('KNOCONV') else []):
                for r0 in range(0, rows, 512):
                    r1 = min(rows, r0 + 512)
                    P.dma("pool", V(dst.ap[l, r0:r1, :], dst.buf), V(src.ap[l, r0:r1, :], src.buf))

        cst = P.sb([128, 6, 128], BF16, "cst")
        P.dma("pool", cst, cst_d)
        ident, m_le, m_lt, m_gt, tinc, bones = (cst[:, i, :] for i in range(6))
        ident_f = P.sb([128, 128], F32, "identf")
        P.dma("sp", ident_f, V(cst_d.ap[:, 0, :], cst_d.buf))
        negm = P.sb([128, 128], F32, "negm")
        P.dma("sp", negm, neg_d)
        cosT = P.sb([128, NB, 32], F32, "cos")
        sinT = P.sb([128, NB, 32], F32, "sin")
        P.dma("sp", cosT, cos_d)
        P.dma("sp", sinT, sin_d)
        ones_bf = P.sb([128, 128], BF16, "ones")
        P.memset("dve", ones_bf, 1.0)
        bandm = P.sb([128, 2, 128], BF16, "bandm")
        P.copy("dve", bandm[:, 0, :], m_gt)
        P.copy("dve", bandm[:, 1, :], m_le)
        epsb = P.sb([128, 1], F32, "epsb")
        P.memset("dve", epsb, EPS)
        onesb = P.sb([128, 1], F32, "onesb")
        P.memset("dve", onesb, 1.0)
        gains = P.sb([128, NL, 4, HD], F32, "gains")
        P.dma("sp", gains, gains_d)
        for l in (range(NL) if 'g' not in SKIP else []):
            P.ts("dve", gains[:, l, 0, :], gains[:, l, 0, :], 0.125, ALU.mult)
            P.ts("dve", gains[:, l, 2, :], gains[:, l, 2, :], 0.125, ALU.mult)
        esink = P.sb([128, NL, 4], F32, "esink")
        P.dma("sp", esink, sinks_d)
        if 'e' not in SKIP: P.act(esink, esink, AF.Exp)
        gout = P.sb([128, NL, 8], F32, "gout")
        P.dma("sp", gout, gout_d)
        ln1 = P.sb([128, NL, 8], F32, "ln1")
        ln2 = P.sb([128, NL, 8], F32, "ln2")
        P.dma("sp", ln1, ln1_d)
        P.dma("sp", ln2, ln2_d)
        bmod = P.sb([128, NL, 48], F32, "bmod")
        P.dma("sp", bmod, bmod_d)
        cact = P.sb([128, 8, NBC], F32, "cact")
        P.dma("sp", cact, cT_d)
        if 's' not in SKIP: P.act(cact, cact, AF.Silu)
        mod = P.sb([128, NL, 48, NBC], F32, "mod")
        A1 = P.sb([128, NL, 8, NBC], F32, "A1")
        A2 = P.sb([128, NL, 8, NBC], F32, "A2")

        PS = [P.ps(f"ps{i}") for i in range(8)]
        rr = {}

        def psum(pool):
            k = rr.get(pool, 0)
            rr[pool] = (k + 1) % len(pool)
            return PS[pool[k]]

        xT = [[P.sb([128, 512], F32, f"x{c}_{t}") for t in range(NT)] for c in range(8)]
        rstd = P.sb([128, 512], F32, "rstd")
        hT = [P.sb([128, 512], BF16, f"h{c}") for c in range(8)]
        sqt = [P.sb([128, 512], BF16, f"sq{i}") for i in range(2)]
        tmpf = [P.sb([128, 512], F32, f"tmpf{i}") for i in range(2)]

        ARENA = 113600
        P.arena_init(ARENA)

        wm = [P.ar([128, 8, 128], F32, f"wm{i}") for i in range(2)]
        it = 0
        for l in (layers if not os.environ.get('KNOMOD') else []):
            for j in range(48):
                w = wm[it % 2]
                it += 1
                P.dma("sp", w, V(wmod_d.ap[l, :, j * 128:(j + 1) * 128].rearrange("(k p) c -> p k c", p=128), wmod_d.buf))
                pm = psum((0, 1))
                for k in range(8):
                    P.mm(pm[:, 0:NBC], w[:, k, :], cact[:, k, :], start=(k == 0), stop=(k == 7))
                P.ts("dve", mod[:, l, j, :], pm[:, 0:NBC], bmod[:, l, j:j + 1], ALU.add)
        for l in (layers if 'a' not in SKIP else []):
            for c in range(8):
                P.ts("dve", A1[:, l, c, :], mod[:, l, 8 + c, :], 1.0, ALU.add, ln1[:, l, c:c + 1], ALU.mult)
                P.ts("dve", A2[:, l, c, :], mod[:, l, 32 + c, :], 1.0, ALU.add, ln2[:, l, c:c + 1], ALU.mult)
        P.barrier()

        def B1(l, c, b): return mod[:, l, 0 + c, b:b + 1]
        def G1(l, c, b): return mod[:, l, 16 + c, b:b + 1]
        def B2(l, c, b): return mod[:, l, 24 + c, b:b + 1]
        def G2(l, c, b): return mod[:, l, 40 + c, b:b + 1]

        P.arena_reset()
        kT = [P.ar([128, 2, 512], BF16, f"kT{t}") for t in range(NT)]
        kbT = [[P.ar([128, 512], BF16, f"kb{p}_{t}") for t in range(NT)] for p in range(2)]
        vall = [P.ar([128, 4, 448], BF16, f"v{t}") for t in range(NT)]
        qT = P.ar([128, 8, 512], BF16, "qT")
        qbT = [P.ar([128, 512], BF16, f"qb{p}") for p in range(2)]
        nqbT = [P.ar([128, 512], BF16, f"nqb{p}") for p in range(2)]
        wi = P.ar([128, 4, 4], F32, "wi")
        u_sq, u_t1 = tmpf
        u_ys = [P.ar([128, 512], F32, f"u_y{i}") for i in range(2)]
        u_rs = [P.ar([128, 512], F32, f"u_r{i}") for i in range(2)]
        u_os = [P.ar([128, 512], BF16, f"u_o{i}") for i in range(2)]
        st_sss = [P.ar([128, 8], F32, f"st_ss{i}") for i in range(2)]
        st_rss = [P.ar([128, 8], F32, f"st_rs{i}") for i in range(2)]
        rp = {"i": 0}
        wA = P.ar([128, 8, 512], BF16, "wA")
        wB = P.ar([128, 8, 260], BF16, "wB")
        wo_s = [P.ar([128, 8, 128], BF16, f"wo{i}") for i in range(2)]
        sc = P.ar([128, L], F32, "sc")
        rel = [P.ar([128, 512], BF16, "rel0")] * 2
        msk = P.ar([128, L], BF16, "msk")
        junk = P.ar([128, L], U8, "junk")
        mT = P.ar([128, NB, 512], U8, "mT")
        bs = {k: P.ar([128, 1], F32, "bs_" + k) for k in ("lo", "w0", "mid", "cnt", "t", "hi")}
        pT = [P.ar([128, 512], BF16, f"pT{i}") for i in range(3)]
        spb = [P.ar([128, 512], BF16, f"sp{i}") for i in range(2)]
        Rb = P.ar([128, 512], BF16, "Rb")
        on = [u_ys[0], u_rs[0]]
        rden = P.ar([128, 512], F32, "rden")
        ef = rden
        oT = [P.ar([128, 512], BF16, f"o{c}") for c in range(8)]
        att_bytes = P.arena_off
        P.arena_reset()
        actT = [P.ar([128, 512], BF16, f"a{f}") for f in range(NFF)]
        sg = [P.ar([128, 512], F32, f"sg{i}") for i in range(2)]
        wgu = [P.ar([128, 8, 2, 256], BF16, f"wgu{i}") for i in range(2)]
        wdn = [P.ar([128, 2, 1024], BF16, f"wdn{i}") for i in range(2)]
        xstage = P.ar([128, 1024], F32, "xstage")

        def wslice(wb, l, cols0, ncols):
            return V(wb.ap[l, :, cols0:cols0 + ncols].rearrange("(k p) c -> p k c", p=128), wb.buf)

        def norm_stats(t):
            pm = psum((0, 1))
            for c in range(8):
                s = sqt[c % 2]
                P.act(s, xT[c][t], AF.Square)
                P.mm(pm, ones_bf, s, start=(c == 0), stop=(c == 7))
            P.act(tmpf[0], pm, AF.Sqrt, scale=1.0 / D, bias=epsb)
            P.recip(rstd, tmpf[0])

        def make_h(t, A, Bf, l, b):
            norm_stats(t)
            for c in range(8):
                tm = tmpf[c % 2]
                P.tt("pool", tm, xT[c][t], rstd, ALU.mult)
                P.act(hT[c], tm, AF.Identity, scale=A[:, l, c, b:b + 1], bias=Bf(l, c, b))

        def hview(v, w, perm):
            nh = w // 64
            if perm:
                return V(v.ap[:, :w].rearrange("p (i s two d) -> p s i two d", s=2, two=2, d=32), v.buf)
            return V(v.ap[:, :w].rearrange("p (s i two d) -> p s i two d", s=2, two=2, d=32), v.buf)

        def rope_norm(pu, nh, tb_glob, gsegs, perm):
            w = nh * 64
            rp["i"] ^= 1
            u_y, u_r, u_o, st_ss, st_rs = u_ys[rp["i"]], u_rs[rp["i"]], u_os[rp["i"]], st_sss[rp["i"]], st_rss[rp["i"]]
            if any(g is not None for (_, _, g) in gsegs):
                P.act(u_sq[:, :w], pu[:, :w], AF.Square)
                P.reduce(st_ss[:, :nh], V(u_sq.ap[:, :w].rearrange("p (h d) -> p h d", d=64), u_sq.buf), ALU.add)
                P.act(st_rs[:, :nh], st_ss[:, :nh], AF.Sqrt, scale=1.0 / HD, bias=epsb)
                P.recip(st_rs[:, :nh], st_rs[:, :nh])
            for (h0, h1, g) in gsegs:
                n = h1 - h0
                src = V(pu.ap[:, h0 * 64:h1 * 64].rearrange("p (h d) -> p h d", d=64), pu.buf)
                dst = V(u_y.ap[:, h0 * 64:h1 * 64].rearrange("p (h d) -> p h d", d=64), u_y.buf)
                if g is None:
                    P.copy("act", dst, src)
                else:
                    P.tt("dve", dst, src, V(st_rs.ap[:, h0:h1].unsqueeze(2).to_broadcast([128, n, 64]), st_rs.buf), ALU.mult)
                    P.tt("pool", dst, dst, V(g.ap.unsqueeze(1).to_broadcast([128, n, 64]), g.buf), ALU.mult)
            y5 = hview(u_y, w, False)
            t5 = hview(u_t1, w, False)
            r5 = hview(u_r, w, False)
            o5 = hview(u_o, w, perm)
            hh = nh // 2
            cb = V(cosT.ap[:, tb_glob, :].unsqueeze(1).unsqueeze(1).unsqueeze(1).to_broadcast([128, 2, hh, 2, 32]), cosT.buf)
            sb_ = V(sinT.ap[:, tb_glob, :].unsqueeze(1).unsqueeze(1).to_broadcast([128, 2, hh, 32]), sinT.buf)
            for s_ in range(2):
                P.tt("dve", t5[:, s_], y5[:, s_], cb[:, s_], ALU.mult)
            P.tt("pool", r5[:, :, :, 0, :], y5[:, :, :, 1, :], sb_, ALU.mult)
            P.tt("pool", r5[:, :, :, 1, :], y5[:, :, :, 0, :], sb_, ALU.mult)
            P.tt("dve", o5[:, :, :, 0, :], t5[:, :, :, 0, :], r5[:, :, :, 0, :], ALU.subtract)
            P.tt("dve", o5[:, :, :, 1, :], t5[:, :, :, 1, :], r5[:, :, :, 1, :], ALU.add)
            return u_o

        def transpose_to(dst3, nchunks, u_o):
            ptr = psum((6, 7))
            ptb = V(ptr.ap.bitcast(BF16), ptr.buf)
            for i in range(nchunks):
                P.tr(ptb[:, i * 128:(i + 1) * 128], u_o[:, i * 128:(i + 1) * 128], ident)
            P.copy("act", dst3, V(ptb.ap[:, :nchunks * 128].rearrange("p (i q) -> p i q", q=128), ptb.buf))

        def stage_kv(l, b):
            P.dma("sp", wB[:, :, 0:256], wslice(win_b, l, O_KB, 256))
            for t in range(NT):
                make_h(t, A1, B1, l, b)
                for p in range(2):
                    pm = psum((0, 1))
                    for k in range(8):
                        P.mm(pm, wB[:, k, p * 128:(p + 1) * 128], hT[k], start=(k == 0), stop=(k == 7))
                    P.copy("act", kbT[p][t], pm)
                o = 0
                for (c0, n) in ((O_KA, 64), (O_KI, 64), (O_KC, 128)):
                    P.dma("sp", wA[:, :, o:o + n], wslice(win_b, l, c0, n))
                    o += n
                for tb in range(4):
                    pk = psum((2, 3))
                    for k in range(8):
                        P.mm(pk[:, :256], hT[k][:, tb * 128:(tb + 1) * 128], wA[:, k, 0:256], start=(k == 0), stop=(k == 7))
                    uo = rope_norm(pk, 4, t * 4 + tb, [(0, 1, gains[:, l, 1, :]), (1, 2, None), (2, 4, gains[:, l, 3, :])], False)
                    transpose_to(kT[t][:, :, tb * 128:(tb + 1) * 128], 2, uo)
                o = 0
                for (c0, n) in ((O_VA, 64), (O_VB, 256), (O_VC, 128)):
                    P.dma("sp", wA[:, :, o:o + n], wslice(win_b, l, c0, n))
                    o += n
                for tb in range(4):
                    pv = psum((4, 5))
                    for k in range(8):
                        P.mm(pv[:, :448], hT[k][:, tb * 128:(tb + 1) * 128], wA[:, k, 0:448], start=(k == 0), stop=(k == 7))
                    P.copy("act", vall[t][:, tb, :], pv[:, :448])

        def stage_q(l, b, G):
            make_h(G, A1, B1, l, b)
            P.dma("sp", wB[:, :, 0:256], wslice(win_b, l, O_QB, 256))
            P.dma("sp", wB[:, :, 256:260], wslice(win_b, l, O_WI, 4))
            for p in range(2):
                pm = psum((0, 1))
                for k in range(8):
                    P.mm(pm, wB[:, k, p * 128:(p + 1) * 128], hT[k], start=(k == 0), stop=(k == 7))
                P.act(qbT[p], pm, AF.Copy, scale=0.125)
                P.act(nqbT[p], pm, AF.Copy, scale=-0.125)
            for tb in range(4):
                pw = psum((0, 1))
                for k in range(8):
                    P.mm(pw[:, 0:4], hT[k][:, tb * 128:(tb + 1) * 128], wB[:, k, 256:260], start=(k == 0), stop=(k == 7))
                P.act(wi[:, tb, :], pw[:, 0:4], AF.Copy, scale=1.0 / 16.0)
            P.dma("sp", wA[:, :, 0:256], wslice(win_b, l, O_QA, 256))
            P.dma("sp", wA[:, :, 256:512], wslice(win_b, l, O_QI, 256))
            for tb in range(4):
                pa = psum((2, 3))
                for k in range(8):
                    P.mm(pa, hT[k][:, tb * 128:(tb + 1) * 128], wA[:, k, :], start=(k == 0), stop=(k == 7))
                uo = rope_norm(pa, 8, G * 4 + tb, [(0, 4, gains[:, l, 0, :]), (4, 8, None)], True)
                transpose_to(qT[:, 0:4, tb * 128:(tb + 1) * 128], 4, uo)
            P.dma("sp", wA, wslice(win_b, l, O_QC, 512))
            for tb in range(4):
                pc = psum((4, 5))
                for k in range(8):
                    P.mm(pc, hT[k][:, tb * 128:(tb + 1) * 128], wA[:, k, :], start=(k == 0), stop=(k == 7))
                uo = rope_norm(pc, 8, G * 4 + tb, [(0, 8, gains[:, l, 2, :])], True)
                transpose_to(qT[:, 4:8, tb * 128:(tb + 1) * 128], 4, uo)

        def keyblk(j):
            return j // 4, (j % 4) * 128

        LO = slice(0, 64)
        HI = slice(64, 128)

        def topk_block(G, nn):
            n0 = 4 * G
            n = n0 + nn
            K = (n + 1) * 128
            qsl = slice(nn * 128, (nn + 1) * 128)
            if K <= NSEL:
                for j in range(n + 1):
                    P.copy("pool", mT[:, j, qsl], ones_bf if j < n else m_le)
                return
            for kc in range((K + 511) // 512):
                k0 = kc * 512
                kw = min(512, K - k0)
                for h in range(4):
                    pm = PS[2 + h % 2]
                    P.mm(pm[:, :kw], qT[HI, h, qsl], kT[kc][HI, 0, 0:kw])
                    r = rel[h % 2]
                    P.act(r[:, :kw], pm[:, :kw], AF.Relu)
                    if h == 0:
                        P.ts("dve", sc[:, k0:k0 + kw], r[:, :kw], wi[:, nn, 0:1], ALU.mult)
                    else:
                        P.stt(sc[:, k0:k0 + kw], r[:, :kw], wi[:, nn, h:h + 1], sc[:, k0:k0 + kw], ALU.mult, ALU.add)
            P.reduce(bs["hi"], sc[:, :K], ALU.max)
            P.reduce(bs["lo"], sc[:, :K], ALU.min)
            P.tt("dve", bs["w0"], bs["hi"], bs["lo"], ALU.subtract)
            P.tt("dve", sc[:, n * 128:K], sc[:, n * 128:K], negm, ALU.add)
            for i in range(1, NIT + 1):
                f = 2.0 ** (-i)
                P.stt(bs["mid"], bs["w0"], f, bs["lo"], ALU.mult, ALU.add)
                P.ts("dve", junk[:, :K], sc[:, :K], bs["mid"], ALU.is_ge, 0.0, ALU.add, accum=bs["cnt"])
                P.ts("dve", bs["t"], bs["cnt"], float(NSEL), ALU.is_ge, f, ALU.mult)
                P.stt(bs["lo"], bs["t"], bs["w0"], bs["lo"], ALU.mult, ALU.add)
            P.ts("dve", msk[:, :K], sc[:, :K], bs["lo"], ALU.is_ge)

        def mask_transposes(G, nn):
            n = 4 * G + nn
            K = (n + 1) * 128
            qsl = slice(nn * 128, (nn + 1) * 128)
            if K <= NSEL:
                return
            for j0 in range(0, n + 1, 8):
                j1 = min(n + 1, j0 + 8)
                ptr = psum((6, 7))
                ptb = V(ptr.ap.bitcast(BF16), ptr.buf)
                for j in range(j0, j1):
                    P.tr(ptb[:, (j - j0) * 128:(j - j0 + 1) * 128], msk[:, j * 128:(j + 1) * 128], ident)
                P.copy("act", mT[:, j0:j1, qsl],
                       V(ptb.ap[:, :(j1 - j0) * 128].rearrange("p (i q) -> p i q", q=128), ptb.buf))

        def norm_heads(l, chunks):
            for i, c in enumerate(chunks):
                s = sqt[i % 2]
                P.act(s, on[i], AF.Square)
                pm = psum((6, 7))
                P.mm(pm, bones, s)
                P.act(rden, pm, AF.Sqrt, scale=1.0 / HD, bias=epsb)
                P.recip(rden, rden)
                P.stt(oT[c], on[i], gout[:, l, c:c + 1], rden, ALU.mult, ALU.mult)

        def dsa(G, l):
            n0 = 4 * G
            jmax = n0 + 3
            po = [PS[0], PS[1]]
            pd = [PS[2], PS[3]]
            tiles = [(j, h) for j in range(jmax + 1) for h in range(4)]

            def geom(j):
                qs = max(0, j - n0) * 128
                tk, ko = keyblk(j)
                return qs, 512 - qs, tk, ko

            def stage1(i):
                j, h = tiles[i]
                qs, N, tk, ko = geom(j)
                pz = psum((4, 5))
                P.mm(pz[:, :N], kT[tk][LO, 0, ko:ko + 128], qT[LO, h, qs:512])
                pt = pT[i % 3]
                P.act(pt[:, :N], pz[:, :N], AF.Exp)
                P.tt("pool", pt[:, :N], pt[:, :N], mT[:, j, qs:512], ALU.mult)

            def stage2(i):
                j, h = tiles[i]
                qs, N, tk, ko = geom(j)
                pr, half = h // 2, h % 2
                hs = HI if half else LO
                pt = pT[i % 3]
                P.mm(po[pr][hs, qs:512], vall[tk][:, j % 4, 0:64], pt[:, :N], start=(j == 0), stop=(j == jmax))
                P.mm(pd[pr][hs, qs:512], ones_bf[:, 0:64], pt[:, :N], start=(j == 0), stop=(j == jmax))

            nt_ = len(tiles)
            for i in range(nt_ + 1):
                if i < nt_:
                    stage1(i)
                if i >= 1:
                    stage2(i - 1)
            for pr in range(2):
                P.recip(rden, pd[pr])
                P.tt("dve", on[pr], po[pr], rden, ALU.mult)
            norm_heads(l, [0, 1])

        def sb_head(G, h):
            n0 = 4 * G
            jmax = n0 + 3
            po = [PS[0], PS[1]]
            pr, half = h // 2, h % 2
            hs = HI if half else LO
            P.memset("pool", Rb, 0.0)

            def geom(j):
                qs = max(0, j - n0) * 128
                tk, ko = keyblk(j)
                return qs, 512 - qs, tk, ko, j >= n0

            def stage1(j):
                qs, N, tk, ko, diag = geom(j)
                pz = psum((4, 5))
                P.mm(pz[:, :N], kbT[pr][tk][hs, ko:ko + 128], qbT[pr][hs, qs:512])
                s = spb[j % 2]
                P.act(ef[:, :N], pz[:, :N], AF.Exp)
                P.act(s[:, :N], ef[:, :N], AF.Ln, bias=onesb)
                if diag:
                    P.tt("pool", s[:, 0:128], s[:, 0:128], m_lt, ALU.mult)

            def stage2(j):
                qs, N, tk, ko, diag = geom(j)
                s = spb[j % 2]
                pc = psum((6, 7))
                P.mm(pc[:, :N], kbT[pr][tk][hs, ko:ko + 128], nqbT[pr][hs, qs:512], start=True, stop=False)
                P.mm(pc[:, :N], tinc, s[:, :N], start=False, stop=(j == jmax))
                if j < jmax:
                    P.mm(pc[:, :N], ones_bf, Rb[:, qs:512], start=False, stop=True)
                a = pT[j % 3]
                P.act(a[:, :N], pc[:, :N], AF.Exp, scale=-1.0)
                if diag:
                    P.tt("pool", a[:, 0:128], a[:, 0:128], m_lt, ALU.mult)
                if j > 0:
                    P.tt("pool", Rb[:, qs:512], Rb[:, qs:512], s[:, :N], ALU.add)

            def stage3(j):
                qs, N, tk, ko, diag = geom(j)
                P.mm(po[pr][hs, qs:512], vall[tk][:, j % 4, 64 + h * 64:64 + (h + 1) * 64], pT[j % 3][:, :N],
                     start=(j == jmax), stop=(j == 0))

            for step in range(jmax, -3, -1):
                if step >= 0:
                    stage1(step)
                if 0 <= step + 1 <= jmax:
                    stage2(step + 1)
                if 0 <= step + 2 <= jmax:
                    stage3(step + 2)

        def sb_finish(l):
            for pr in range(2):
                P.copy("dve", on[pr], PS[pr])
            norm_heads(l, [2, 3])

        def swa(G, l):
            n0 = 4 * G
            for g in range(2):
                gs = HI if g else LO
                po = [PS[0], PS[1]]
                pd = [PS[2], PS[3]]
                for nn in range(4):
                    n = n0 + nn
                    kbs = [n - 1, n] if n > 0 else [n]
                    pz = [PS[4], PS[5]]
                    pts = {}
                    for kb in kbs:
                        i = kb - n + 1
                        tk, ko = keyblk(kb)
                        P.mm(V(pz[i].ap.rearrange("p (c q) -> p c q", q=128), pz[i].buf),
                             kT[tk][gs, 1, ko:ko + 128], qT[gs, 4:8, nn * 128:(nn + 1) * 128])
                        pt = pT[(nn * 2 + i) % 3]
                        pts[kb] = pt
                        P.act(pt, pz[i], AF.Exp)
                        P.tt("pool", V(pt.ap.rearrange("p (a q) -> p a q", q=128), pt.buf),
                             V(pt.ap.rearrange("p (a q) -> p a q", q=128), pt.buf),
                             V(bandm.ap[:, i, :].unsqueeze(1).to_broadcast([128, 4, 128]), bandm.buf), ALU.mult)
                    bank = nn // 2
                    off = (nn % 2) * 256
                    for half in range(2):
                        hs = HI if half else LO
                        for kb in kbs:
                            tk, ko = keyblk(kb)
                            rhs = V(pts[kb].ap.rearrange("p (c s q) -> p s c q", s=2, q=128)[:, half], pts[kb].buf)
                            outv = V(po[bank].ap[hs, off:off + 256].rearrange("p (c q) -> p c q", q=128), po[bank].buf)
                            outd = V(pd[bank].ap[hs, off:off + 256].rearrange("p (c q) -> p c q", q=128), pd[bank].buf)
                            P.mm(outv, vall[tk][:, kb % 4, 320 + g * 64:320 + (g + 1) * 64], rhs,
                                 start=(kb == kbs[0]), stop=(kb == kbs[-1]))
                            P.mm(outd, ones_bf[:, 0:64], rhs, start=(kb == kbs[0]), stop=(kb == kbs[-1]))
                for cc in range(2):
                    ch = 2 * g + cc
                    for bank in range(2):
                        den = V(pd[bank].ap.rearrange("p (n c q) -> p n c q", n=2, c=2)[:, :, cc, :], pd[bank].buf)
                        num = V(po[bank].ap.rearrange("p (n c q) -> p n c q", n=2, c=2)[:, :, cc, :], po[bank].buf)
                        rd = V(rden.ap[:, 0:256].rearrange("p (n q) -> p n q", q=128), rden.buf)
                        P.ts("dve", rd, den, esink[:, l, ch:ch + 1], ALU.add)
                        P.recip(rd, rd)
                        dst = V(on[cc].ap[:, bank * 256:(bank + 1) * 256].rearrange("p (n q) -> p n q", q=128), on[cc].buf)
                        P.tt("dve", dst, num, rd, ALU.mult)
                norm_heads(l, [4 + 2 * g, 5 + 2 * g])

        def out_proj(l, b, G):
            for co in range(8):
                w = wo_s[co % 2]
                P.dma("sp", w, wslice(wo_b, l, co * 128, 128))
                pm = psum((4, 5))
                for k in range(8):
                    P.mm(pm, w[:, k, :], oT[k], start=(k == 0), stop=(k == 7))
                P.stt(xT[co][G], pm, G1(l, co, b), xT[co][G], ALU.mult, ALU.add)

        def ffn(l, b):
            for t in range(NT):
                make_h(t, A2, B2, l, b)
                for f2 in range(NFF // 2):
                    w = wgu[f2 % 2]
                    P.dma("sp", w[:, :, 0, :], wslice(wg_b, l, f2 * 256, 256))
                    P.dma("sp", w[:, :, 1, :], wslice(wu_b, l, f2 * 256, 256))
                    for ff in range(2):
                        f = f2 * 2 + ff
                        pg = psum((0, 1))
                        pu = psum((2, 3))
                        for k in range(8):
                            P.mm(pg, w[:, k, 0, ff * 128:(ff + 1) * 128], hT[k], start=(k == 0), stop=(k == 7))
                        for k in range(8):
                            P.mm(pu, w[:, k, 1, ff * 128:(ff + 1) * 128], hT[k], start=(k == 0), stop=(k == 7))
                        P.act(sg[f % 2], pg, AF.Silu)
                        P.tt("dve", actT[f], sg[f % 2], pu, ALU.mult)
                pacc = [PS[4], PS[5], PS[6], PS[7]]
                for half in range(2):
                    for f2 in range(NFF // 2):
                        w = wdn[f2 % 2]
                        P.dma("sp", w, V(wd_b.ap[l, f2 * 256:(f2 + 1) * 256, :].rearrange("(k p) c -> p k c", p=128), wd_b.buf))
                        for ff in range(2):
                            f = f2 * 2 + ff
                            for c4 in range(4):
                                co = half * 4 + c4
                                P.mm(pacc[c4], w[:, ff, co * 128:(co + 1) * 128], actT[f], start=(f == 0), stop=(f == NFF - 1))
                    for c4 in range(4):
                        co = half * 4 + c4
                        P.stt(xT[co][t], pacc[c4], G2(l, co, b), xT[co][t], ALU.mult, ALU.add)

        for b in range(NBC):
            P.barrier()
            for t in (range(NT) if 'x' not in SKIP else []):
                for tb in range(4):
                    r0 = (t * 4 + tb) * 128
                    P.dma("sp", xstage, V(x_d.ap[b, r0:r0 + 128, :], x_d.buf))
                    for c4 in range(0, 8, 4):
                        pm = psum((0, 1, 2, 3))
                        for c in range(c4, c4 + 4):
                            P.tr(pm[:, (c - c4) * 128:(c - c4 + 1) * 128], xstage[:, c * 128:(c + 1) * 128], ident_f)
                        for c in range(c4, c4 + 4):
                            P.copy("act" if c % 2 else "dve", xT[c][t][:, tb * 128:(tb + 1) * 128],
                                   pm[:, (c - c4) * 128:(c - c4 + 1) * 128])
            for l in layers:
                P.barrier()
                if STOP < 1: continue
                stage_kv(l, b)
                for G in range(NT):
                    if STOP < 2: continue
                    stage_q(l, b, G)
                    if STOP < 3: continue
                    for nn in range(4):
                        topk_block(G, nn)
                        sb_head(G, nn)
                        mask_transposes(G, nn)
                    sb_finish(l)
                    swa(G, l)
                    dsa(G, l)
                    out_proj(l, b, G)
                P.barrier()
                if STOP < 8: continue
                ffn(l, b)
            P.barrier()
            for t in range(NT):
                for tb in range(4):
                    r0 = (t * 4 + tb) * 128
                    for c4 in (range(0, 8, 4) if 'y' not in SKIP else []):
                        pm = psum((0, 1, 2, 3))
                        for c in range(c4, c4 + 4):
                            P.tr(pm[:, (c - c4) * 128:(c - c4 + 1) * 128], xT[c][t][:, tb * 128:(tb + 1) * 128], ident_f)
                        P.copy("act" if c4 else "dve", xstage[:, c4 * 128:(c4 + 4) * 128], pm)
                    P.dma("sp", V(y_d.ap[b, r0:r0 + 128, :], y_d.buf), xstage, out_is_ext=True)

        print("arena bytes: att", att_bytes, "max", P.arena_max, "ops", {k: len(v) for k, v in P.ops.items()})
        block = es.enter_context(nc.Block())
        P.emit(block)
    return nc


_CACHE = {}


def _host_consts(L):
    NB = L // 128
    kk = np.arange(128)[:, None]
    qq = np.arange(128)[None, :]
    cst = np.zeros((128, 6, 128), np.float32)
    cst[:, 0] = np.eye(128)
    cst[:, 1] = (kk <= qq)
    cst[:, 2] = (kk < qq)
    cst[:, 3] = (kk > qq)
    cst[:, 4] = (kk >= qq)
    cst[:, 5] = ((kk // 64) == (qq // 64))
    negm = np.where(qq <= kk, 0.0, NEG).astype(np.float32)
    inv = (1.0 / (np.float32(10000.0) ** (np.arange(0, 64, 2, dtype=np.float32) / np.float32(64)))).astype(np.float32)
    ang = np.arange(L, dtype=np.float32)[:, None] * inv[None, :]
    cos = np.cos(ang).astype(np.float32).reshape(NB, 128, 32).transpose(1, 0, 2)
    sin = np.sin(ang).astype(np.float32).reshape(NB, 128, 32).transpose(1, 0, 2)
    return cst, negm, np.ascontiguousarray(cos), np.ascontiguousarray(sin)


def _run(inputs, L, NBC, ncores, layers):
    key = (L, NBC, tuple(layers))
    if key not in _CACHE:
        _CACHE[key] = build_program(L, NBC, layers)
    nc = _CACHE[key]
    f = lambda a: np.ascontiguousarray(np.asarray(a, dtype=np.float32))
    cst, negm, cos, sin = _host_consts(L)
    x = f(inputs["x"])
    c = f(inputs["c"])
    NL = 4
    def featT(a):
        a = f(a)
        return np.ascontiguousarray(a.reshape(NL, -1, 128).transpose(2, 0, 1))
    gains = np.stack([f(inputs["qn_a"]), f(inputs["kn_a"]), f(inputs["qn_c"]), f(inputs["kn_c"])], axis=1)
    gainsB = np.ascontiguousarray(np.broadcast_to(gains[None], (128, NL, 4, 64)))
    sk = f(inputs["sinks"])
    sinksT = np.zeros((128, NL, 4), np.float32)
    for ch in range(4):
        sinksT[:64, :, ch] = sk[:, ch][None, :]
        sinksT[64:, :, ch] = sk[:, 4 + ch][None, :]
    common = {
        "ln1T": featT(inputs["ln1"]), "ln2T": featT(inputs["ln2"]), "bmodT": featT(inputs["b_mod"]),
        "w_mod": f(inputs["w_mod"]), "w_in": f(inputs["w_in"]), "w_o": f(inputs["w_o"]),
        "w_gate": f(inputs["w_gate"]), "w_up": f(inputs["w_up"]), "w_down": f(inputs["w_down"]),
        "gainsB": gainsB, "sinksT": sinksT, "goutT": featT(f(inputs["g_out"]).reshape(NL, -1)),
        "cosT": cos, "sinT": sin, "consts": cst, "negm": negm,
    }
    in_maps = []
    for i in range(ncores):
        cb = c[i * NBC:(i + 1) * NBC]
        cT = np.ascontiguousarray(cb.reshape(NBC, 8, 128).transpose(2, 1, 0))
        m = dict(common)
        m["x"] = np.ascontiguousarray(x[i * NBC:(i + 1) * NBC])
        m["cT"] = cT
        in_maps.append(m)
    res = run_bass_kernel_spmd(nc, in_maps, core_ids=list(range(ncores)))
    return np.concatenate([r["y"] for r in res.results], axis=0)


def kernel(x, c, ln1, ln2, w_mod, b_mod, w_in, qn_a, kn_a, qn_c, kn_c, sinks, g_out, w_o, w_gate, w_up, w_down):
    inputs = dict(x=x, c=c, ln1=ln1, ln2=ln2, w_mod=w_mod, b_mod=b_mod, w_in=w_in, qn_a=qn_a, kn_a=kn_a,
                  qn_c=qn_c, kn_c=kn_c, sinks=sinks, g_out=g_out, w_o=w_o, w_gate=w_gate, w_up=w_up, w_down=w_down)
    B, L = x.shape[0], x.shape[1]
    out = _run(inputs, L, B // 8, 8, [0, 1, 2, 3])
    return out.astype(np.float32)
```

```python
import contextlib
import numpy as np
import concourse.bass as bass
import concourse.mybir as mybir
from concourse.bass_utils import run_bass_kernel_spmd

F32 = mybir.dt.float32
BF16 = mybir.dt.bfloat16
U32 = mybir.dt.uint32
FP8 = mybir.dt.float8e4
U8 = mybir.dt.uint8
AF = mybir.ActivationFunctionType
ALU = mybir.AluOpType
AX = mybir.AxisListType

D = 1024
DFF = 2816
NFF = DFF // 128
INC = 2244
HD = 64
EPS = 1e-6
NEG = -1.0e30
NIT = 17
import os
STOP = int(os.environ.get('KSTOP', '9'))
SKIP = os.environ.get('KSKIP', '')
O_QA, O_KA, O_VA, O_QI, O_KI, O_WI, O_QB, O_KB, O_VB, O_QC, O_KC, O_VC = (
    0, 256, 320, 384, 640, 704, 708, 964, 1220, 1476, 1988, 2116)


class Buf:
    __slots__ = ("name", "w", "r", "dsem", "excl")

    def __init__(self, name, excl=False):
        self.name = name
        self.excl = excl
        self.w = None
        self.r = []
        self.dsem = None


class V:
    __slots__ = ("ap", "buf")

    def __init__(self, ap, buf):
        self.ap = ap
        self.buf = buf

    def __getitem__(self, idx):
        return V(self.ap[idx], self.buf)


class Prog:
    ENG = ("pe", "dve", "act", "pool", "sp")

    def __init__(self, nc, es):
        self.nc = nc
        self.es = es
        self.ops = {e: [] for e in self.ENG}
        self.cnt = {}
        self.seen = {e: {} for e in self.ENG}
        self.sems = {}
        for e in ("pe", "dve", "act", "pool"):
            self.sems[e] = es.enter_context(nc.semaphore("S_" + e))
            self.cnt[e] = 0
        self.ndsem = 0
        self.nt = 0
        self.out_waits = []

    def sb(self, shape, dt, name=None):
        self.nt += 1
        name = name or f"t{self.nt}"
        t = self.es.enter_context(self.nc.sbuf_tensor(name + f"_{self.nt}", list(shape), dt))
        return V(t[:], Buf(name))

    def ps(self, name):
        self.nt += 1
        t = self.es.enter_context(self.nc.psum_tensor(name + f"_{self.nt}", [128, 512], F32))
        return V(t[:], Buf(name, excl=True))

    def arena_init(self, nbytes):
        t = self.es.enter_context(self.nc.sbuf_tensor("arena", [128, nbytes // 4], F32))
        self.arena = t[:]
        self.arena_n = nbytes
        self.arena_off = 0
        self.arena_max = 0

    def arena_reset(self):
        self.arena_off = 0

    def ar(self, shape, dt, name):
        n = 1
        for d in shape[1:]:
            n *= d
        esz = {F32: 4, BF16: 2, U32: 4, FP8: 1, U8: 1}[dt]
        nb = (n * esz + 3) // 4 * 4
        off = self.arena_off
        self.arena_off += nb
        self.arena_max = max(self.arena_max, self.arena_off)
        assert self.arena_off <= self.arena_n, (name, self.arena_off, self.arena_n)
        ap = self.arena[:, off // 4:(off + nb) // 4]
        if dt != F32:
            ap = ap.bitcast(dt)
        ap = ap[:, 0:n]
        if len(shape) == 3:
            ap = ap.rearrange("p (a b) -> p a b", b=shape[2])
        elif len(shape) == 4:
            ap = ap.rearrange("p (a b c) -> p a b c", b=shape[2], c=shape[3])
        return V(ap, Buf(name))

    def dram(self, name, shape, dt):
        t = self.nc.dram_tensor(name, list(shape), dt, kind="Internal")
        return V(t.ap(), Buf(name))

    def dsem(self, buf):
        if buf.dsem is None:
            key = f"d{self.ndsem}"
            self.ndsem += 1
            self.sems[key] = self.es.enter_context(self.nc.semaphore("D_" + key))
            self.cnt[key] = 0
            buf.dsem = key
        return buf.dsem

    def _deps(self, eng, reads, writes):
        need = {}
        for b in reads:
            if b.w is not None:
                k, v = b.w
                need[k] = max(need.get(k, 0), v)
            if b.excl:
                for (k, v) in b.r:
                    if k != eng:
                        need[k] = max(need.get(k, 0), v)
        for b in writes:
            if b.w is not None:
                k, v = b.w
                need[k] = max(need.get(k, 0), v)
            for (k, v) in b.r:
                need[k] = max(need.get(k, 0), v)
        waits = []
        seen = self.seen[eng]
        for k, v in need.items():
            if k == eng:
                if eng == "pe":
                    continue
            if seen.get(k, 0) >= v:
                continue
            seen[k] = v
            waits.append((k, v))
        return waits

    def _mark(self, key, val, reads, writes):
        for b in reads:
            b.r.append((key, val))
            if len(b.r) > 24:
                m = {}
                for (k, v) in b.r:
                    m[k] = max(m.get(k, 0), v)
                b.r = list(m.items())
        for b in writes:
            b.w = (key, val)
            b.r = []

    def op(self, eng, fn, reads, writes):
        rb = [x.buf for x in reads]
        wb = [x.buf for x in writes]
        waits = self._deps(eng, rb, wb)
        self.cnt[eng] += 1
        self._mark(eng, self.cnt[eng], rb, wb)
        self.ops[eng].append((fn, waits, (eng, 1)))

    def dma(self, q, out, in_, out_is_ext=False):
        rb = [in_.buf]
        wb = [out.buf]
        waits = self._deps(q, rb, wb)
        key = self.dsem(out.buf)
        self.cnt[key] += 16
        self._mark(key, self.cnt[key], rb, wb)
        oa, ia = out.ap, in_.ap
        self.ops[q].append((lambda e: e.dma_start(out=oa, in_=ia), waits, (key, 16)))
        if out_is_ext:
            self.out_waits.append((key, self.cnt[key]))

    def barrier(self):
        for e in self.ENG:
            waits = []
            for k, v in self.cnt.items():
                if k == e or v == 0:
                    continue
                if self.seen[e].get(k, 0) >= v:
                    continue
                self.seen[e][k] = v
                waits.append((k, v))
            if waits:
                self.ops[e].append((None, waits, None))

    def emit(self, block):
        nc = self.nc
        engs = {"pe": (block.tensor, None), "dve": (block.vector, None), "act": (block.scalar, None),
                "pool": (block.gpsimd, None), "sp": (block.sync, None)}
        self.ops["sp"].append((None, list(self.out_waits), None))
        for name in self.ENG:
            ops = self.ops[name]
            sems = self.sems

            def body(e, ops=ops):
                for (fn, waits, inc) in ops:
                    for (k, v) in waits:
                        e.wait_ge(sems[k], v)
                    if fn is not None:
                        ins = fn(e)
                        ins.then_inc(sems[inc[0]], inc[1])
            engs[name][0](body)

    def mm(self, out, lhsT, rhs, start=True, stop=True):
        o, l, r = out.ap, lhsT.ap, rhs.ap
        self.op("pe", lambda e: e.matmul(o, lhsT=l, rhs=r, start=start, stop=stop, skip_group_check=True),
                [lhsT, rhs], [out])

    def tr(self, out, in_, ident):
        o, i, d = out.ap, in_.ap, ident.ap
        self.op("pe", lambda e: e.transpose(o, i, d), [in_, ident], [out])

    def act(self, out, in_, func, scale=1.0, bias=None, extra=()):
        o, i = out.ap, in_.ap
        rd = [in_] + list(extra)
        sc = scale.ap if isinstance(scale, V) else scale
        if isinstance(scale, V):
            rd.append(scale)
        if isinstance(bias, V):
            rd.append(bias)
            bi = bias.ap
            self.op("act", lambda e: e.activation(out=o, in_=i, func=func, bias=bi, scale=sc), rd, [out])
        elif bias is None:
            self.op("act", lambda e: e.activation(out=o, in_=i, func=func, scale=sc), rd, [out])
        else:
            self.op("act", lambda e: e.activation(out=o, in_=i, func=func, bias=bias, scale=sc), rd, [out])

    def ts(self, eng, out, in0, s1, op0, s2=None, op1=None, accum=None):
        o, i = out.ap, in0.ap
        rd = [in0]
        a1 = s1.ap if isinstance(s1, V) else s1
        a2 = s2.ap if isinstance(s2, V) else s2
        if isinstance(s1, V):
            rd.append(s1)
        if isinstance(s2, V):
            rd.append(s2)
        wr = [out]
        kw = {}
        if op1 is not None:
            kw["op1"] = op1
        if accum is not None:
            kw["accum_out"] = accum.ap
            wr.append(accum)
        self.op(eng, lambda e: e.tensor_scalar(out=o, in0=i, scalar1=a1, scalar2=a2, op0=op0, **kw), rd, wr)

    def tt(self, eng, out, in0, in1, op):
        o, a, b = out.ap, in0.ap, in1.ap
        self.op(eng, lambda e: e.tensor_tensor(out=o, in0=a, in1=b, op=op), [in0, in1], [out])

    def stt(self, out, in0, scalar, in1, op0, op1):
        o, a, b = out.ap, in0.ap, in1.ap
        rd = [in0, in1]
        s = scalar.ap if isinstance(scalar, V) else scalar
        if isinstance(scalar, V):
            rd.append(scalar)
        self.op("dve", lambda e: e.scalar_tensor_tensor(out=o, in0=a, scalar=s, in1=b, op0=op0, op1=op1), rd, [out])

    def copy(self, eng, out, in_):
        o, i = out.ap, in_.ap
        if eng == "act":
            self.op("act", lambda e: e.activation(out=o, in_=i, func=AF.Copy), [in_], [out])
        else:
            self.op(eng, lambda e: e.tensor_copy(out=o, in_=i), [in_], [out])

    def memset(self, eng, out, val):
        o = out.ap
        self.op(eng, lambda e: e.memset(o, val), [], [out])

    def recip(self, out, in_):
        o, i = out.ap, in_.ap
        self.op("dve", lambda e: e.reciprocal(out=o, in_=i), [in_], [out])

    def reduce(self, out, in_, op):
        o, i = out.ap, in_.ap
        self.op("dve", lambda e: e.tensor_reduce(out=o, in_=i, axis=AX.X, op=op), [in_], [out])


def build_program(L, NBC, layers):
    NB = L // 128
    NT = L // 512
    NSEL = min(256, L // 4)
    NL = 4
    nc = bass.Bass("TRN2", target_bir_lowering=False)
    es = contextlib.ExitStack()
    with es:
        P = Prog(nc, es)

        def ext(name, shape, dt=F32, out=False):
            t = nc.dram_tensor(name, list(shape), dt, kind="ExternalOutput" if out else "ExternalInput")
            return V(t.ap(), Buf(name))

        x_d = ext("x", [NBC, L, D])
        y_d = ext("y", [NBC, L, D], out=True)
        cT_d = ext("cT", [128, 8, NBC])
        ln1_d = ext("ln1T", [128, NL, 8])
        ln2_d = ext("ln2T", [128, NL, 8])
        bmod_d = ext("bmodT", [128, NL, 48])
        wmod_d = ext("w_mod", [NL, D, 6 * D])
        win_d = ext("w_in", [NL, D, INC])
        wo_d = ext("w_o", [NL, D, D])
        wg_d = ext("w_gate", [NL, D, DFF])
        wu_d = ext("w_up", [NL, D, DFF])
        wd_d = ext("w_down", [NL, DFF, D])
        gains_d = ext("gainsB", [128, NL, 4, HD])
        sinks_d = ext("sinksT", [128, NL, 4])
        gout_d = ext("goutT", [128, NL, 8])
        cos_d = ext("cosT", [128, NB, 32])
        sin_d = ext("sinT", [128, NB, 32])
        cst_d = ext("consts", [128, 6, 128])
        neg_d = ext("negm", [128, 128])

        win_b = P.dram("win_b", [NL, D, INC], BF16)
        wo_b = P.dram("wo_b", [NL, D, D], BF16)
        wg_b = P.dram("wg_b", [NL, NFF // 2, 128, 8, 256], BF16)
        wu_b = P.dram("wu_b", [NL, NFF // 2, 128, 8, 256], BF16)
        wd_b = P.dram("wd_b", [NL, DFF, D], BF16)
        for (dst, src) in ((wg_b, wg_d), (wu_b, wu_d)):
            for l in (layers if not os.environ.get('KNOCONV') else []):
                for f2 in range(NFF // 2):
                    P.dma("pool", V(dst.ap[l, f2], dst.buf),
                          V(src.ap[l, :, f2 * 256:(f2 + 1) * 256].rearrange("(k p) c -> p k c", p=128), src.buf))
        for (dst, src, rows) in ((win_b, win_d, D), (wo_b, wo_d, D), (wd_b, wd_d, DFF)):
            for l in (layers if not os.environ.get('KNOCONV') else []):
                for r0 in range(0, rows, 512):
                    r1 = min(rows, r0 + 512)
                    P.dma("pool", V(dst.ap[l, r0:r1, :], dst.buf), V(src.ap[l, r0:r1, :], src.buf))

        cst = P.sb([128, 6, 128], BF16, "cst")
        P.dma("pool", cst, cst_d)
        ident, m_le, m_lt, m_gt, tinc, bones = (cst[:, i, :] for i in range(6))
        ident_f = P.sb([128, 128], F32, "identf")
        P.dma("sp", ident_f, V(cst_d.ap[:, 0, :], cst_d.buf))
        negm = P.sb([128, 128], F32, "negm")
        P.dma("sp", negm, neg_d)
        cosT = P.sb([128, NB, 32], F32, "cos")
        sinT = P.sb([128, NB, 32], F32, "sin")
        P.dma("sp", cosT, cos_d)
        P.dma("sp", sinT, sin_d)
        ones_bf = P.sb([128, 128], BF16, "ones")
        P.memset("dve", ones_bf, 1.0)
        bandm = P.sb([128, 2, 128], BF16, "bandm")
        P.copy("dve", bandm[:, 0, :], m_gt)
        P.copy("dve", bandm[:, 1, :], m_le)
        epsb = P.sb([128, 1], F32, "epsb")
        P.memset("dve", epsb, EPS)
        onesb = P.sb([128, 1], F32, "onesb")
        P.memset("dve", onesb, 1.0)
        gains = P.sb([128, NL, 4, HD], F32, "gains")
        P.dma("sp", gains, gains_d)
        for l in (range(NL) if 'g' not in SKIP else []):
            P.ts("dve", gains[:, l, 0, :], gains[:, l, 0, :], 0.125, ALU.mult)
            P.ts("dve", gains[:, l, 2, :], gains[:, l, 2, :], 0.125, ALU.mult)
        esink = P.sb([128, NL, 4], F32, "esink")
        P.dma("sp", esink, sinks_d)
        if 'e' not in SKIP: P.act(esink, esink, AF.Exp)
        gout = P.sb([128, NL, 8], F32, "gout")
        P.dma("sp", gout, gout_d)
        ln1 = P.sb([128, NL, 8], F32, "ln1")
        ln2 = P.sb([128, NL, 8], F32, "ln2")
        P.dma("sp", ln1, ln1_d)
        P.dma("sp", ln2, ln2_d)
        bmod = P.sb([128, NL, 48], F32, "bmod")
        P.dma("sp", bmod, bmod_d)
        cact = P.sb([128, 8, NBC], F32, "cact")
        P.dma("sp", cact, cT_d)
        if 's' not in SKIP: P.act(cact, cact, AF.Silu)
        mod = P.sb([128, NL, 48, NBC], F32, "mod")
        A1 = P.sb([128, NL, 8, NBC], F32, "A1")
        A2 = P.sb([128, NL, 8, NBC], F32, "A2")

        PS = [P.ps(f"ps{i}") for i in range(8)]
        rr = {}

        def psum(pool):
            k = rr.get(pool, 0)
            rr[pool] = (k + 1) % len(pool)
            return PS[pool[k]]

        xT = [[P.sb([128, 512], F32, f"x{c}_{t}") for t in range(NT)] for c in range(8)]
        rstd = P.sb([128, 512], F32, "rstd")
        hT = [P.sb([128, 512], BF16, f"h{c}") for c in range(8)]
        sqt = [P.sb([128, 512], BF16, f"sq{i}") for i in range(2)]
        tmpf = [P.sb([128, 512], F32, f"tmpf{i}") for i in range(2)]

        ARENA = 113600
        P.arena_init(ARENA)

        wm = [P.ar([128, 8, 128], F32, f"wm{i}") for i in range(2)]
        it = 0
        for l in (layers if not os.environ.get('KNOMOD') else []):
            for j in range(48):
                w = wm[it % 2]
                it += 1
                P.dma("sp", w, V(wmod_d.ap[l, :, j * 128:(j + 1) * 128].rearrange("(k p) c -> p k c", p=128), wmod_d.buf))
                pm = psum((0, 1))
                for k in range(8):
                    P.mm(pm[:, 0:NBC], w[:, k, :], cact[:, k, :], start=(k == 0), stop=(k == 7))
                P.ts("dve", mod[:, l, j, :], pm[:, 0:NBC], bmod[:, l, j:j + 1], ALU.add)
        for l in (layers if 'a' not in SKIP else []):
            for c in range(8):
                P.ts("dve", A1[:, l, c, :], mod[:, l, 8 + c, :], 1.0, ALU.add, ln1[:, l, c:c + 1], ALU.mult)
                P.ts("dve", A2[:, l, c, :], mod[:, l, 32 + c, :], 1.0, ALU.add, ln2[:, l, c:c + 1], ALU.mult)
        P.barrier()

        def B1(l, c, b): return mod[:, l, 0 + c, b:b + 1]
        def G1(l, c, b): return mod[:, l, 16 + c, b:b + 1]
        def B2(l, c, b): return mod[:, l, 24 + c, b:b + 1]
        def G2(l, c, b): return mod[:, l, 40 + c, b:b + 1]

        P.arena_reset()
        kT = [P.ar([128, 2, 512], BF16, f"kT{t}") for t in range(NT)]
        kbT = [[P.ar([128, 512], BF16, f"kb{p}_{t}") for t in range(NT)] for p in range(2)]
        vall = [P.ar([128, 4, 448], BF16, f"v{t}") for t in range(NT)]
        qT = P.ar([128, 8, 512], BF16, "qT")
        qbT = [P.ar([128, 512], BF16, f"qb{p}") for p in range(2)]
        nqbT = [P.ar([128, 512], BF16, f"nqb{p}") for p in range(2)]
        wi = P.ar([128, 4, 4], F32, "wi")
        u_sq, u_t1 = tmpf
        u_ys = [P.ar([128, 512], F32, f"u_y{i}") for i in range(2)]
        u_rs = [P.ar([128, 512], F32, f"u_r{i}") for i in range(2)]
        u_os = [P.ar([128, 512], BF16, f"u_o{i}") for i in range(2)]
        st_sss = [P.ar([128, 8], F32, f"st_ss{i}") for i in range(2)]
        st_rss = [P.ar([128, 8], F32, f"st_rs{i}") for i in range(2)]
        rp = {"i": 0}
        wA = P.ar([128, 8, 512], BF16, "wA")
        wB = P.ar([128, 8, 260], BF16, "wB")
        wo_s = [P.ar([128, 8, 128], BF16, f"wo{i}") for i in range(2)]
        sc = P.ar([128, L], F32, "sc")
        rel = [P.ar([128, 512], BF16, f"rel{i}") for i in range(2)]
        msk = P.ar([128, L], BF16, "msk")
        junk = P.ar([128, L], U8, "junk")
        mT = P.ar([128, NB, 512], U8, "mT")
        bs = {k: P.ar([128, 1], F32, "bs_" + k) for k in ("lo", "w0", "mid", "cnt", "t", "hi")}
        pT = [P.ar([128, 512], BF16, f"pT{i}") for i in range(3)]
        spb = [P.ar([128, 512], BF16, f"sp{i}") for i in range(2)]
        Rb = P.ar([128, 512], BF16, "Rb")
        on = [u_ys[0], u_rs[0]]
        rden = P.ar([128, 512], F32, "rden")
        ef = rden
        oT = [P.ar([128, 512], BF16, f"o{c}") for c in range(8)]
        att_bytes = P.arena_off
        P.arena_reset()
        actT = [P.ar([128, 512], BF16, f"a{f}") for f in range(NFF)]
        sg = [P.ar([128, 512], F32, f"sg{i}") for i in range(2)]
        wgu = [P.ar([128, 8, 2, 256], BF16, f"wgu{i}") for i in range(2)]
        wdn = [P.ar([128, 2, 1024], BF16, f"wdn{i}") for i in range(2)]
        xstage = P.ar([128, 1024], F32, "xstage")

        def wslice(wb, l, cols0, ncols):
            return V(wb.ap[l, :, cols0:cols0 + ncols].rearrange("(k p) c -> p k c", p=128), wb.buf)

        def norm_stats(t):
            pm = psum((0, 1))
            for c in range(8):
                s = sqt[c % 2]
                P.act(s, xT[c][t], AF.Square)
                P.mm(pm, ones_bf, s, start=(c == 0), stop=(c == 7))
            P.act(tmpf[0], pm, AF.Sqrt, scale=1.0 / D, bias=epsb)
            P.recip(rstd, tmpf[0])

        def make_h(t, A, Bf, l, b):
            norm_stats(t)
            for c in range(8):
                tm = tmpf[c % 2]
                P.tt("pool", tm, xT[c][t], rstd, ALU.mult)
                P.act(hT[c], tm, AF.Identity, scale=A[:, l, c, b:b + 1], bias=Bf(l, c, b))

        def hview(v, w, perm):
            nh = w // 64
            if perm:
                return V(v.ap[:, :w].rearrange("p (i s two d) -> p s i two d", s=2, two=2, d=32), v.buf)
            return V(v.ap[:, :w].rearrange("p (s i two d) -> p s i two d", s=2, two=2, d=32), v.buf)

        def rope_norm(pu, nh, tb_glob, gsegs, perm):
            w = nh * 64
            rp["i"] ^= 1
            u_y, u_r, u_o, st_ss, st_rs = u_ys[rp["i"]], u_rs[rp["i"]], u_os[rp["i"]], st_sss[rp["i"]], st_rss[rp["i"]]
            if any(g is not None for (_, _, g) in gsegs):
                P.act(u_sq[:, :w], pu[:, :w], AF.Square)
                P.reduce(st_ss[:, :nh], V(u_sq.ap[:, :w].rearrange("p (h d) -> p h d", d=64), u_sq.buf), ALU.add)
                P.act(st_rs[:, :nh], st_ss[:, :nh], AF.Sqrt, scale=1.0 / HD, bias=epsb)
                P.recip(st_rs[:, :nh], st_rs[:, :nh])
            for (h0, h1, g) in gsegs:
                n = h1 - h0
                src = V(pu.ap[:, h0 * 64:h1 * 64].rearrange("p (h d) -> p h d", d=64), pu.buf)
                dst = V(u_y.ap[:, h0 * 64:h1 * 64].rearrange("p (h d) -> p h d", d=64), u_y.buf)
                if g is None:
                    P.copy("act", dst, src)
                else:
                    P.tt("dve", dst, src, V(st_rs.ap[:, h0:h1].unsqueeze(2).to_broadcast([128, n, 64]), st_rs.buf), ALU.mult)
                    P.tt("pool", dst, dst, V(g.ap.unsqueeze(1).to_broadcast([128, n, 64]), g.buf), ALU.mult)
            y5 = hview(u_y, w, False)
            t5 = hview(u_t1, w, False)
            r5 = hview(u_r, w, False)
            o5 = hview(u_o, w, perm)
            hh = nh // 2
            cb = V(cosT.ap[:, tb_glob, :].unsqueeze(1).unsqueeze(1).unsqueeze(1).to_broadcast([128, 2, hh, 2, 32]), cosT.buf)
            sb_ = V(sinT.ap[:, tb_glob, :].unsqueeze(1).unsqueeze(1).to_broadcast([128, 2, hh, 32]), sinT.buf)
            for s_ in range(2):
                P.tt("dve", t5[:, s_], y5[:, s_], cb[:, s_], ALU.mult)
            P.tt("pool", r5[:, :, :, 0, :], y5[:, :, :, 1, :], sb_, ALU.mult)
            P.tt("pool", r5[:, :, :, 1, :], y5[:, :, :, 0, :], sb_, ALU.mult)
            P.tt("dve", o5[:, :, :, 0, :], t5[:, :, :, 0, :], r5[:, :, :, 0, :], ALU.subtract)
            P.tt("dve", o5[:, :, :, 1, :], t5[:, :, :, 1, :], r5[:, :, :, 1, :], ALU.add)
            return u_o

        def transpose_to(dst3, nchunks, u_o):
            ptr = psum((6, 7))
            ptb = V(ptr.ap.bitcast(BF16), ptr.buf)
            for i in range(nchunks):
                P.tr(ptb[:, i * 128:(i + 1) * 128], u_o[:, i * 128:(i + 1) * 128], ident)
            P.copy("act", dst3, V(ptb.ap[:, :nchunks * 128].rearrange("p (i q) -> p i q", q=128), ptb.buf))

        def stage_kv(l, b):
            P.dma("sp", wB[:, :, 0:256], wslice(win_b, l, O_KB, 256))
            for t in range(NT):
                make_h(t, A1, B1, l, b)
                for p in range(2):
                    pm = psum((0, 1))
                    for k in range(8):
                        P.mm(pm, wB[:, k, p * 128:(p + 1) * 128], hT[k], start=(k == 0), stop=(k == 7))
                    P.copy("act", kbT[p][t], pm)
                o = 0
                for (c0, n) in ((O_KA, 64), (O_KI, 64), (O_KC, 128)):
                    P.dma("sp", wA[:, :, o:o + n], wslice(win_b, l, c0, n))
                    o += n
                for tb in range(4):
                    pk = psum((2, 3))
                    for k in range(8):
                        P.mm(pk[:, :256], hT[k][:, tb * 128:(tb + 1) * 128], wA[:, k, 0:256], start=(k == 0), stop=(k == 7))
                    uo = rope_norm(pk, 4, t * 4 + tb, [(0, 1, gains[:, l, 1, :]), (1, 2, None), (2, 4, gains[:, l, 3, :])], False)
                    transpose_to(kT[t][:, :, tb * 128:(tb + 1) * 128], 2, uo)
                o = 0
                for (c0, n) in ((O_VA, 64), (O_VB, 256), (O_VC, 128)):
                    P.dma("sp", wA[:, :, o:o + n], wslice(win_b, l, c0, n))
                    o += n
                for tb in range(4):
                    pv = psum((4, 5))
                    for k in range(8):
                        P.mm(pv[:, :448], hT[k][:, tb * 128:(tb + 1) * 128], wA[:, k, 0:448], start=(k == 0), stop=(k == 7))
                    P.copy("act", vall[t][:, tb, :], pv[:, :448])

        def stage_q(l, b, G):
            make_h(G, A1, B1, l, b)
            P.dma("sp", wB[:, :, 0:256], wslice(win_b, l, O_QB, 256))
            P.dma("sp", wB[:, :, 256:260], wslice(win_b, l, O_WI, 4))
            for p in range(2):
                pm = psum((0, 1))
                for k in range(8):
                    P.mm(pm, wB[:, k, p * 128:(p + 1) * 128], hT[k], start=(k == 0), stop=(k == 7))
                P.act(qbT[p], pm, AF.Copy, scale=0.125)
                P.act(nqbT[p], pm, AF.Copy, scale=-0.125)
            for tb in range(4):
                pw = psum((0, 1))
                for k in range(8):
                    P.mm(pw[:, 0:4], hT[k][:, tb * 128:(tb + 1) * 128], wB[:, k, 256:260], start=(k == 0), stop=(k == 7))
                P.act(wi[:, tb, :], pw[:, 0:4], AF.Copy, scale=1.0 / 16.0)
            P.dma("sp", wA[:, :, 0:256], wslice(win_b, l, O_QA, 256))
            P.dma("sp", wA[:, :, 256:512], wslice(win_b, l, O_QI, 256))
            for tb in range(4):
                pa = psum((2, 3))
                for k in range(8):
                    P.mm(pa, hT[k][:, tb * 128:(tb + 1) * 128], wA[:, k, :], start=(k == 0), stop=(k == 7))
                uo = rope_norm(pa, 8, G * 4 + tb, [(0, 4, gains[:, l, 0, :]), (4, 8, None)], True)
                transpose_to(qT[:, 0:4, tb * 128:(tb + 1) * 128], 4, uo)
            P.dma("sp", wA, wslice(win_b, l, O_QC, 512))
            for tb in range(4):
                pc = psum((4, 5))
                for k in range(8):
                    P.mm(pc, hT[k][:, tb * 128:(tb + 1) * 128], wA[:, k, :], start=(k == 0), stop=(k == 7))
                uo = rope_norm(pc, 8, G * 4 + tb, [(0, 8, gains[:, l, 2, :])], True)
                transpose_to(qT[:, 4:8, tb * 128:(tb + 1) * 128], 4, uo)

        def keyblk(j):
            return j // 4, (j % 4) * 128

        LO = slice(0, 64)
        HI = slice(64, 128)

        def topk_block(G, nn):
            n0 = 4 * G
            n = n0 + nn
            K = (n + 1) * 128
            qsl = slice(nn * 128, (nn + 1) * 128)
            if K <= NSEL:
                for j in range(n + 1):
                    P.copy("pool", mT[:, j, qsl], ones_bf if j < n else m_le)
                return
            for kc in range((K + 511) // 512):
                k0 = kc * 512
                kw = min(512, K - k0)
                for h in range(4):
                    pm = PS[2 + h % 2]
                    P.mm(pm[:, :kw], qT[HI, h, qsl], kT[kc][HI, 0, 0:kw])
                    r = rel[h % 2]
                    P.act(r[:, :kw], pm[:, :kw], AF.Relu)
                    if h == 0:
                        P.ts("dve", sc[:, k0:k0 + kw], r[:, :kw], wi[:, nn, 0:1], ALU.mult)
                    else:
                        P.stt(sc[:, k0:k0 + kw], r[:, :kw], wi[:, nn, h:h + 1], sc[:, k0:k0 + kw], ALU.mult, ALU.add)
            P.reduce(bs["hi"], sc[:, :K], ALU.max)
            P.reduce(bs["lo"], sc[:, :K], ALU.min)
            P.tt("dve", bs["w0"], bs["hi"], bs["lo"], ALU.subtract)
            P.tt("dve", sc[:, n * 128:K], sc[:, n * 128:K], negm, ALU.add)
            for i in range(1, NIT + 1):
                f = 2.0 ** (-i)
                P.stt(bs["mid"], bs["w0"], f, bs["lo"], ALU.mult, ALU.add)
                P.ts("dve", junk[:, :K], sc[:, :K], bs["mid"], ALU.is_ge, 0.0, ALU.add, accum=bs["cnt"])
                P.ts("dve", bs["t"], bs["cnt"], float(NSEL), ALU.is_ge, f, ALU.mult)
                P.stt(bs["lo"], bs["t"], bs["w0"], bs["lo"], ALU.mult, ALU.add)
            P.ts("dve", msk[:, :K], sc[:, :K], bs["lo"], ALU.is_ge)

        def mask_transposes(G, nn):
            n = 4 * G + nn
            K = (n + 1) * 128
            qsl = slice(nn * 128, (nn + 1) * 128)
            if K <= NSEL:
                return
            for j0 in range(0, n + 1, 8):
                j1 = min(n + 1, j0 + 8)
                ptr = psum((6, 7))
                ptb = V(ptr.ap.bitcast(BF16), ptr.buf)
                for j in range(j0, j1):
                    P.tr(ptb[:, (j - j0) * 128:(j - j0 + 1) * 128], msk[:, j * 128:(j + 1) * 128], ident)
                P.copy("act", mT[:, j0:j1, qsl],
                       V(ptb.ap[:, :(j1 - j0) * 128].rearrange("p (i q) -> p i q", q=128), ptb.buf))

        def norm_heads(l, chunks):
            for i, c in enumerate(chunks):
                s = sqt[i % 2]
                P.act(s, on[i], AF.Square)
                pm = psum((6, 7))
                P.mm(pm, bones, s)
                P.act(rden, pm, AF.Sqrt, scale=1.0 / HD, bias=epsb)
                P.recip(rden, rden)
                P.stt(oT[c], on[i], gout[:, l, c:c + 1], rden, ALU.mult, ALU.mult)

        def dsa(G, l):
            n0 = 4 * G
            jmax = n0 + 3
            po = [PS[0], PS[1]]
            pd = [PS[2], PS[3]]
            tiles = [(j, h) for j in range(jmax + 1) for h in range(4)]

            def geom(j):
                qs = max(0, j - n0) * 128
                tk, ko = keyblk(j)
                return qs, 512 - qs, tk, ko

            def stage1(i):
                j, h = tiles[i]
                qs, N, tk, ko = geom(j)
                pz = psum((4, 5))
                P.mm(pz[:, :N], kT[tk][LO, 0, ko:ko + 128], qT[LO, h, qs:512])
                pt = pT[i % 3]
                P.act(pt[:, :N], pz[:, :N], AF.Exp)
                P.tt("pool", pt[:, :N], pt[:, :N], mT[:, j, qs:512], ALU.mult)

            def stage2(i):
                j, h = tiles[i]
                qs, N, tk, ko = geom(j)
                pr, half = h // 2, h % 2
                hs = HI if half else LO
                pt = pT[i % 3]
                P.mm(po[pr][hs, qs:512], vall[tk][:, j % 4, 0:64], pt[:, :N], start=(j == 0), stop=(j == jmax))
                P.mm(pd[pr][hs, qs:512], ones_bf[:, 0:64], pt[:, :N], start=(j == 0), stop=(j == jmax))

            nt_ = len(tiles)
            for i in range(nt_ + 1):
                if i < nt_:
                    stage1(i)
                if i >= 1:
                    stage2(i - 1)
            for pr in range(2):
                P.recip(rden, pd[pr])
                P.tt("dve", on[pr], po[pr], rden, ALU.mult)
            norm_heads(l, [0, 1])

        def sb_head(G, h):
            n0 = 4 * G
            jmax = n0 + 3
            po = [PS[0], PS[1]]
            pr, half = h // 2, h % 2
            hs = HI if half else LO
            P.memset("pool", Rb, 0.0)

            def geom(j):
                qs = max(0, j - n0) * 128
                tk, ko = keyblk(j)
                return qs, 512 - qs, tk, ko, j >= n0

            def stage1(j):
                qs, N, tk, ko, diag = geom(j)
                pz = psum((4, 5))
                P.mm(pz[:, :N], kbT[pr][tk][hs, ko:ko + 128], qbT[pr][hs, qs:512])
                s = spb[j % 2]
                P.act(ef[:, :N], pz[:, :N], AF.Exp)
                P.act(s[:, :N], ef[:, :N], AF.Ln, bias=onesb)
                if diag:
                    P.tt("pool", s[:, 0:128], s[:, 0:128], m_lt, ALU.mult)

            def stage2(j):
                qs, N, tk, ko, diag = geom(j)
                s = spb[j % 2]
                pc = psum((6, 7))
                P.mm(pc[:, :N], kbT[pr][tk][hs, ko:ko + 128], nqbT[pr][hs, qs:512], start=True, stop=False)
                P.mm(pc[:, :N], tinc, s[:, :N], start=False, stop=(j == jmax))
                if j < jmax:
                    P.mm(pc[:, :N], ones_bf, Rb[:, qs:512], start=False, stop=True)
                a = pT[j % 3]
                P.act(a[:, :N], pc[:, :N], AF.Exp, scale=-1.0)
                if diag:
                    P.tt("pool", a[:, 0:128], a[:, 0:128], m_lt, ALU.mult)
                if j > 0:
                    P.tt("pool", Rb[:, qs:512], Rb[:, qs:512], s[:, :N], ALU.add)

            def stage3(j):
                qs, N, tk, ko, diag = geom(j)
                P.mm(po[pr][hs, qs:512], vall[tk][:, j % 4, 64 + h * 64:64 + (h + 1) * 64], pT[j % 3][:, :N],
                     start=(j == jmax), stop=(j == 0))

            for step in range(jmax, -3, -1):
                if step >= 0:
                    stage1(step)
                if 0 <= step + 1 <= jmax:
                    stage2(step + 1)
                if 0 <= step + 2 <= jmax:
                    stage3(step + 2)

        def sb_finish(l):
            for pr in range(2):
                P.copy("dve", on[pr], PS[pr])
            norm_heads(l, [2, 3])

        def swa(G, l):
            n0 = 4 * G
            for g in range(2):
                gs = HI if g else LO
                po = [PS[0], PS[1]]
                pd = [PS[2], PS[3]]
                for nn in range(4):
                    n = n0 + nn
                    kbs = [n - 1, n] if n > 0 else [n]
                    pz = [PS[4], PS[5]]
                    pts = {}
                    for kb in kbs:
                        i = kb - n + 1
                        tk, ko = keyblk(kb)
                        P.mm(V(pz[i].ap.rearrange("p (c q) -> p c q", q=128), pz[i].buf),
                             kT[tk][gs, 1, ko:ko + 128], qT[gs, 4:8, nn * 128:(nn + 1) * 128])
                        pt = pT[(nn * 2 + i) % 3]
                        pts[kb] = pt
                        P.act(pt, pz[i], AF.Exp)
                        P.tt("pool", V(pt.ap.rearrange("p (a q) -> p a q", q=128), pt.buf),
                             V(pt.ap.rearrange("p (a q) -> p a q", q=128), pt.buf),
                             V(bandm.ap[:, i, :].unsqueeze(1).to_broadcast([128, 4, 128]), bandm.buf), ALU.mult)
                    bank = nn // 2
                    off = (nn % 2) * 256
                    for half in range(2):
                        hs = HI if half else LO
                        for kb in kbs:
                            tk, ko = keyblk(kb)
                            rhs = V(pts[kb].ap.rearrange("p (c s q) -> p s c q", s=2, q=128)[:, half], pts[kb].buf)
                            outv = V(po[bank].ap[hs, off:off + 256].rearrange("p (c q) -> p c q", q=128), po[bank].buf)
                            outd = V(pd[bank].ap[hs, off:off + 256].rearrange("p (c q) -> p c q", q=128), pd[bank].buf)
                            P.mm(outv, vall[tk][:, kb % 4, 320 + g * 64:320 + (g + 1) * 64], rhs,
                                 start=(kb == kbs[0]), stop=(kb == kbs[-1]))
                            P.mm(outd, ones_bf[:, 0:64], rhs, start=(kb == kbs[0]), stop=(kb == kbs[-1]))
                for cc in range(2):
                    ch = 2 * g + cc
                    for bank in range(2):
                        den = V(pd[bank].ap.rearrange("p (n c q) -> p n c q", n=2, c=2)[:, :, cc, :], pd[bank].buf)
                        num = V(po[bank].ap.rearrange("p (n c q) -> p n c q", n=2, c=2)[:, :, cc, :], po[bank].buf)
                        rd = V(rden.ap[:, 0:256].rearrange("p (n q) -> p n q", q=128), rden.buf)
                        P.ts("dve", rd, den, esink[:, l, ch:ch + 1], ALU.add)
                        P.recip(rd, rd)
                        dst = V(on[cc].ap[:, bank * 256:(bank + 1) * 256].rearrange("p (n q) -> p n q", q=128), on[cc].buf)
                        P.tt("dve", dst, num, rd, ALU.mult)
                norm_heads(l, [4 + 2 * g, 5 + 2 * g])

        def out_proj(l, b, G):
            for co in range(8):
                w = wo_s[co % 2]
                P.dma("sp", w, wslice(wo_b, l, co * 128, 128))
                pm = psum((4, 5))
                for k in range(8):
                    P.mm(pm, w[:, k, :], oT[k], start=(k == 0), stop=(k == 7))
                P.stt(xT[co][G], pm, G1(l, co, b), xT[co][G], ALU.mult, ALU.add)

        def ffn(l, b):
            for t in range(NT):
                make_h(t, A2, B2, l, b)
                for f2 in range(NFF // 2):
                    w = wgu[f2 % 2]
                    P.dma("sp", w[:, :, 0, :], V(wg_b.ap[l, f2], wg_b.buf))
                    P.dma("sp", w[:, :, 1, :], V(wu_b.ap[l, f2], wu_b.buf))
                    for ff in range(2):
                        f = f2 * 2 + ff
                        pg = psum((0, 1))
                        pu = psum((2, 3))
                        for k in range(8):
                            P.mm(pg, w[:, k, 0, ff * 128:(ff + 1) * 128], hT[k], start=(k == 0), stop=(k == 7))
                        for k in range(8):
                            P.mm(pu, w[:, k, 1, ff * 128:(ff + 1) * 128], hT[k], start=(k == 0), stop=(k == 7))
                        P.act(sg[f % 2], pg, AF.Silu)
                        P.tt("dve", actT[f], sg[f % 2], pu, ALU.mult)
                for f2 in range(NFF // 2):
                    w = wdn[f2 % 2]
                    P.dma("sp", w, V(wd_b.ap[l, f2 * 256:(f2 + 1) * 256, :].rearrange("(k p) c -> p k c", p=128), wd_b.buf))
                    for ff in range(2):
                        f = f2 * 2 + ff
                        for co in range(8):
                            P.mm(PS[co], w[:, ff, co * 128:(co + 1) * 128], actT[f], start=(f == 0), stop=(f == NFF - 1))
                for co in range(8):
                    P.stt(xT[co][t], PS[co], G2(l, co, b), xT[co][t], ALU.mult, ALU.add)

        for b in range(NBC):
            P.barrier()
            for t in (range(NT) if 'x' not in SKIP else []):
                for tb in range(4):
                    r0 = (t * 4 + tb) * 128
                    P.dma("sp", xstage, V(x_d.ap[b, r0:r0 + 128, :], x_d.buf))
                    for c4 in range(0, 8, 4):
                        pm = psum((0, 1, 2, 3))
                        for c in range(c4, c4 + 4):
                            P.tr(pm[:, (c - c4) * 128:(c - c4 + 1) * 128], xstage[:, c * 128:(c + 1) * 128], ident_f)
                        for c in range(c4, c4 + 4):
                            P.copy("act" if c % 2 else "dve", xT[c][t][:, tb * 128:(tb + 1) * 128],
                                   pm[:, (c - c4) * 128:(c - c4 + 1) * 128])
            for l in layers:
                P.barrier()
                if STOP < 1: continue
                stage_kv(l, b)
                for G in range(NT):
                    if STOP < 2: continue
                    stage_q(l, b, G)
                    if STOP < 3: continue
                    for nn in range(4):
                        topk_block(G, nn)
                        sb_head(G, nn)
                        mask_transposes(G, nn)
                    sb_finish(l)
                    swa(G, l)
                    dsa(G, l)
                    out_proj(l, b, G)
                P.barrier()
                if STOP < 8: continue
                ffn(l, b)
            P.barrier()
            for t in range(NT):
                for tb in range(4):
                    r0 = (t * 4 + tb) * 128
                    for c4 in (range(0, 8, 4) if 'y' not in SKIP else []):
                        pm = psum((0, 1, 2, 3))
                        for c in range(c4, c4 + 4):
                            P.tr(pm[:, (c - c4) * 128:(c - c4 + 1) * 128], xT[c][t][:, tb * 128:(tb + 1) * 128], ident_f)
                        P.copy("act" if c4 else "dve", xstage[:, c4 * 128:(c4 + 4) * 128], pm)
                    P.dma("sp", V(y_d.ap[b, r0:r0 + 128, :], y_d.buf), xstage, out_is_ext=True)

        print("arena bytes: att", att_bytes, "max", P.arena_max, "ops", {k: len(v) for k, v in P.ops.items()})
        block = es.enter_context(nc.Block())
        P.emit(block)
    return nc


_CACHE = {}


def _host_consts(L):
    NB = L // 128
    kk = np.arange(128)[:, None]
    qq = np.arange(128)[None, :]
    cst = np.zeros((128, 6, 128), np.float32)
    cst[:, 0] = np.eye(128)
    cst[:, 1] = (kk <= qq)
    cst[:, 2] = (kk < qq)
    cst[:, 3] = (kk > qq)
    cst[:, 4] = (kk >= qq)
    cst[:, 5] = ((kk // 64) == (qq // 64))
    negm = np.where(qq <= kk, 0.0, NEG).astype(np.float32)
    inv = (1.0 / (np.float32(10000.0) ** (np.arange(0, 64, 2, dtype=np.float32) / np.float32(64)))).astype(np.float32)
    ang = np.arange(L, dtype=np.float32)[:, None] * inv[None, :]
    cos = np.cos(ang).astype(np.float32).reshape(NB, 128, 32).transpose(1, 0, 2)
    sin = np.sin(ang).astype(np.float32).reshape(NB, 128, 32).transpose(1, 0, 2)
    return cst, negm, np.ascontiguousarray(cos), np.ascontiguousarray(sin)


def _run(inputs, L, NBC, ncores, layers):
    key = (L, NBC, tuple(layers))
    if key not in _CACHE:
        _CACHE[key] = build_program(L, NBC, layers)
    nc = _CACHE[key]
    f = lambda a: np.ascontiguousarray(np.asarray(a, dtype=np.float32))
    cst, negm, cos, sin = _host_consts(L)
    x = f(inputs["x"])
    c = f(inputs["c"])
    NL = 4
    def featT(a):
        a = f(a)
        return np.ascontiguousarray(a.reshape(NL, -1, 128).transpose(2, 0, 1))
    gains = np.stack([f(inputs["qn_a"]), f(inputs["kn_a"]), f(inputs["qn_c"]), f(inputs["kn_c"])], axis=1)
    gainsB = np.ascontiguousarray(np.broadcast_to(gains[None], (128, NL, 4, 64)))
    sk = f(inputs["sinks"])
    sinksT = np.zeros((128, NL, 4), np.float32)
    for ch in range(4):
        sinksT[:64, :, ch] = sk[:, ch][None, :]
        sinksT[64:, :, ch] = sk[:, 4 + ch][None, :]
    common = {
        "ln1T": featT(inputs["ln1"]), "ln2T": featT(inputs["ln2"]), "bmodT": featT(inputs["b_mod"]),
        "w_mod": f(inputs["w_mod"]), "w_in": f(inputs["w_in"]), "w_o": f(inputs["w_o"]),
        "w_gate": f(inputs["w_gate"]), "w_up": f(inputs["w_up"]), "w_down": f(inputs["w_down"]),
        "gainsB": gainsB, "sinksT": sinksT, "goutT": featT(f(inputs["g_out"]).reshape(NL, -1)),
        "cosT": cos, "sinT": sin, "consts": cst, "negm": negm,
    }
    in_maps = []
    for i in range(ncores):
        cb = c[i * NBC:(i + 1) * NBC]
        cT = np.ascontiguousarray(cb.reshape(NBC, 8, 128).transpose(2, 1, 0))
        m = dict(common)
        m["x"] = np.ascontiguousarray(x[i * NBC:(i + 1) * NBC])
        m["cT"] = cT
        in_maps.append(m)
    res = run_bass_kernel_spmd(nc, in_maps, core_ids=list(range(ncores)))
    return np.concatenate([r["y"] for r in res.results], axis=0)


def kernel(x, c, ln1, ln2, w_mod, b_mod, w_in, qn_a, kn_a, qn_c, kn_c, sinks, g_out, w_o, w_gate, w_up, w_down):
    inputs = dict(x=x, c=c, ln1=ln1, ln2=ln2, w_mod=w_mod, b_mod=b_mod, w_in=w_in, qn_a=qn_a, kn_a=kn_a,
                  qn_c=qn_c, kn_c=kn_c, sinks=sinks, g_out=g_out, w_o=w_o, w_gate=w_gate, w_up=w_up, w_down=w_down)
    B, L = x.shape[0], x.shape[1]
    out = _run(inputs, L, B // 8, 8, [0, 1, 2, 3])
    return out.astype(np.float32)
```

```python
import contextlib
import numpy as np
import concourse.bass as bass
import concourse.mybir as mybir
from concourse.bass_utils import run_bass_kernel_spmd

F32 = mybir.dt.float32
BF16 = mybir.dt.bfloat16
U32 = mybir.dt.uint32
FP8 = mybir.dt.float8e4
U8 = mybir.dt.uint8
AF = mybir.ActivationFunctionType
ALU = mybir.AluOpType
AX = mybir.AxisListType

D = 1024
DFF = 2816
NFF = DFF // 128
INC = 2244
HD = 64
EPS = 1e-6
NEG = -1.0e30
NIT = 17
import os
STOP = int(os.environ.get('KSTOP', '9'))
SKIP = os.environ.get('KSKIP', '')
O_QA, O_KA, O_VA, O_QI, O_KI, O_WI, O_QB, O_KB, O_VB, O_QC, O_KC, O_VC = (
    0, 256, 320, 384, 640, 704, 708, 964, 1220, 1476, 1988, 2116)


class Buf:
    __slots__ = ("name", "w", "r", "dsem", "excl")

    def __init__(self, name, excl=False):
        self.name = name
        self.excl = excl
        self.w = None
        self.r = []
        self.dsem = None


class V:
    __slots__ = ("ap", "buf")

    def __init__(self, ap, buf):
        self.ap = ap
        self.buf = buf

    def __getitem__(self, idx):
        return V(self.ap[idx], self.buf)


class Prog:
    ENG = ("pe", "dve", "act", "pool", "sp")

    def __init__(self, nc, es):
        self.nc = nc
        self.es = es
        self.ops = {e: [] for e in self.ENG}
        self.cnt = {}
        self.seen = {e: {} for e in self.ENG}
        self.sems = {}
        for e in ("pe", "dve", "act", "pool"):
            self.sems[e] = es.enter_context(nc.semaphore("S_" + e))
            self.cnt[e] = 0
        self.ndsem = 0
        self.nt = 0
        self.out_waits = []

    def sb(self, shape, dt, name=None):
        self.nt += 1
        name = name or f"t{self.nt}"
        t = self.es.enter_context(self.nc.sbuf_tensor(name + f"_{self.nt}", list(shape), dt))
        return V(t[:], Buf(name))

    def ps(self, name):
        self.nt += 1
        t = self.es.enter_context(self.nc.psum_tensor(name + f"_{self.nt}", [128, 512], F32))
        return V(t[:], Buf(name, excl=True))

    def arena_init(self, nbytes):
        t = self.es.enter_context(self.nc.sbuf_tensor("arena", [128, nbytes // 4], F32))
        self.arena = t[:]
        self.arena_n = nbytes
        self.arena_off = 0
        self.arena_max = 0

    def arena_reset(self):
        self.arena_off = 0

    def ar(self, shape, dt, name):
        n = 1
        for d in shape[1:]:
            n *= d
        esz = {F32: 4, BF16: 2, U32: 4, FP8: 1, U8: 1}[dt]
        nb = (n * esz + 3) // 4 * 4
        off = self.arena_off
        self.arena_off += nb
        self.arena_max = max(self.arena_max, self.arena_off)
        assert self.arena_off <= self.arena_n, (name, self.arena_off, self.arena_n)
        ap = self.arena[:, off // 4:(off + nb) // 4]
        if dt != F32:
            ap = ap.bitcast(dt)
        ap = ap[:, 0:n]
        if len(shape) == 3:
            ap = ap.rearrange("p (a b) -> p a b", b=shape[2])
        elif len(shape) == 4:
            ap = ap.rearrange("p (a b c) -> p a b c", b=shape[2], c=shape[3])
        return V(ap, Buf(name))

    def dram(self, name, shape, dt):
        t = self.nc.dram_tensor(name, list(shape), dt, kind="Internal")
        return V(t.ap(), Buf(name))

    def dsem(self, buf):
        if buf.dsem is None:
            key = f"d{self.ndsem}"
            self.ndsem += 1
            self.sems[key] = self.es.enter_context(self.nc.semaphore("D_" + key))
            self.cnt[key] = 0
            buf.dsem = key
        return buf.dsem

    def _deps(self, eng, reads, writes):
        need = {}
        for b in reads:
            if b.w is not None:
                k, v = b.w
                need[k] = max(need.get(k, 0), v)
            if b.excl:
                for (k, v) in b.r:
                    if k != eng:
                        need[k] = max(need.get(k, 0), v)
        for b in writes:
            if b.w is not None:
                k, v = b.w
                need[k] = max(need.get(k, 0), v)
            for (k, v) in b.r:
                need[k] = max(need.get(k, 0), v)
        waits = []
        seen = self.seen[eng]
        for k, v in need.items():
            if k == eng:
                if eng == "pe":
                    continue
            if seen.get(k, 0) >= v:
                continue
            seen[k] = v
            waits.append((k, v))
        return waits

    def _mark(self, key, val, reads, writes):
        for b in reads:
            b.r.append((key, val))
            if len(b.r) > 24:
                m = {}
                for (k, v) in b.r:
                    m[k] = max(m.get(k, 0), v)
                b.r = list(m.items())
        for b in writes:
            b.w = (key, val)
            b.r = []

    def op(self, eng, fn, reads, writes):
        rb = [x.buf for x in reads]
        wb = [x.buf for x in writes]
        waits = self._deps(eng, rb, wb)
        self.cnt[eng] += 1
        self._mark(eng, self.cnt[eng], rb, wb)
        self.ops[eng].append((fn, waits, (eng, 1)))

    def dma(self, q, out, in_, out_is_ext=False):
        rb = [in_.buf]
        wb = [out.buf]
        waits = self._deps(q, rb, wb)
        key = self.dsem(in_.buf if out_is_ext else out.buf)
        self.cnt[key] += 16
        self._mark(key, self.cnt[key], rb, wb)
        oa, ia = out.ap, in_.ap
        self.ops[q].append((lambda e: e.dma_start(out=oa, in_=ia), waits, (key, 16)))
        if out_is_ext:
            self.out_waits.append((key, self.cnt[key]))

    def barrier(self):
        for e in self.ENG:
            waits = []
            for k, v in self.cnt.items():
                if k == e or v == 0:
                    continue
                if self.seen[e].get(k, 0) >= v:
                    continue
                self.seen[e][k] = v
                waits.append((k, v))
            if waits:
                self.ops[e].append((None, waits, None))

    def emit(self, block):
        nc = self.nc
        engs = {"pe": (block.tensor, None), "dve": (block.vector, None), "act": (block.scalar, None),
                "pool": (block.gpsimd, None), "sp": (block.sync, None)}
        self.ops["sp"].append((None, list(self.out_waits), None))
        for name in self.ENG:
            ops = self.ops[name]
            sems = self.sems

            def body(e, ops=ops):
                for (fn, waits, inc) in ops:
                    for (k, v) in waits:
                        e.wait_ge(sems[k], v)
                    if fn is not None:
                        ins = fn(e)
                        ins.then_inc(sems[inc[0]], inc[1])
            engs[name][0](body)

    def mm(self, out, lhsT, rhs, start=True, stop=True):
        o, l, r = out.ap, lhsT.ap, rhs.ap
        self.op("pe", lambda e: e.matmul(o, lhsT=l, rhs=r, start=start, stop=stop, skip_group_check=True),
                [lhsT, rhs], [out])

    def tr(self, out, in_, ident):
        o, i, d = out.ap, in_.ap, ident.ap
        self.op("pe", lambda e: e.transpose(o, i, d), [in_, ident], [out])

    def act(self, out, in_, func, scale=1.0, bias=None, extra=()):
        o, i = out.ap, in_.ap
        rd = [in_] + list(extra)
        sc = scale.ap if isinstance(scale, V) else scale
        if isinstance(scale, V):
            rd.append(scale)
        if isinstance(bias, V):
            rd.append(bias)
            bi = bias.ap
            self.op("act", lambda e: e.activation(out=o, in_=i, func=func, bias=bi, scale=sc), rd, [out])
        elif bias is None:
            self.op("act", lambda e: e.activation(out=o, in_=i, func=func, scale=sc), rd, [out])
        else:
            self.op("act", lambda e: e.activation(out=o, in_=i, func=func, bias=bias, scale=sc), rd, [out])

    def ts(self, eng, out, in0, s1, op0, s2=None, op1=None, accum=None):
        o, i = out.ap, in0.ap
        rd = [in0]
        a1 = s1.ap if isinstance(s1, V) else s1
        a2 = s2.ap if isinstance(s2, V) else s2
        if isinstance(s1, V):
            rd.append(s1)
        if isinstance(s2, V):
            rd.append(s2)
        wr = [out]
        kw = {}
        if op1 is not None:
            kw["op1"] = op1
        if accum is not None:
            kw["accum_out"] = accum.ap
            wr.append(accum)
        self.op(eng, lambda e: e.tensor_scalar(out=o, in0=i, scalar1=a1, scalar2=a2, op0=op0, **kw), rd, wr)

    def tt(self, eng, out, in0, in1, op):
        o, a, b = out.ap, in0.ap, in1.ap
        self.op(eng, lambda e: e.tensor_tensor(out=o, in0=a, in1=b, op=op), [in0, in1], [out])

    def stt(self, out, in0, scalar, in1, op0, op1):
        o, a, b = out.ap, in0.ap, in1.ap
        rd = [in0, in1]
        s = scalar.ap if isinstance(scalar, V) else scalar
        if isinstance(scalar, V):
            rd.append(scalar)
        self.op("dve", lambda e: e.scalar_tensor_tensor(out=o, in0=a, scalar=s, in1=b, op0=op0, op1=op1), rd, [out])

    def copy(self, eng, out, in_):
        o, i = out.ap, in_.ap
        if eng == "act":
            self.op("act", lambda e: e.activation(out=o, in_=i, func=AF.Copy), [in_], [out])
        else:
            self.op(eng, lambda e: e.tensor_copy(out=o, in_=i), [in_], [out])

    def memset(self, eng, out, val):
        o = out.ap
        self.op(eng, lambda e: e.memset(o, val), [], [out])

    def recip(self, out, in_):
        o, i = out.ap, in_.ap
        self.op("dve", lambda e: e.reciprocal(out=o, in_=i), [in_], [out])

    def reduce(self, out, in_, op):
        o, i = out.ap, in_.ap
        self.op("dve", lambda e: e.tensor_reduce(out=o, in_=i, axis=AX.X, op=op), [in_], [out])


def build_program(L, NBC, layers):
    NB = L // 128
    NT = L // 512
    NSEL = min(256, L // 4)
    NL = 4
    nc = bass.Bass("TRN2", target_bir_lowering=False)
    es = contextlib.ExitStack()
    with es:
        P = Prog(nc, es)

        def ext(name, shape, dt=F32, out=False):
            t = nc.dram_tensor(name, list(shape), dt, kind="ExternalOutput" if out else "ExternalInput")
            return V(t.ap(), Buf(name))

        x_d = ext("x", [NBC, L, D])
        y_d = ext("y", [NBC, L, D], out=True)
        cT_d = ext("cT", [128, 8, NBC])
        ln1_d = ext("ln1T", [128, NL, 8])
        ln2_d = ext("ln2T", [128, NL, 8])
        bmod_d = ext("bmodT", [128, NL, 48])
        wmod_d = ext("w_mod", [NL, D, 6 * D])
        win_d = ext("w_in", [NL, D, INC])
        wo_d = ext("w_o", [NL, D, D])
        wg_d = ext("w_gate", [NL, D, DFF])
        wu_d = ext("w_up", [NL, D, DFF])
        wd_d = ext("w_down", [NL, DFF, D])
        gains_d = ext("gainsB", [128, NL, 4, HD])
        sinks_d = ext("sinksT", [128, NL, 4])
        gout_d = ext("goutT", [128, NL, 8])
        cos_d = ext("cosT", [128, NB, 32])
        sin_d = ext("sinT", [128, NB, 32])
        cst_d = ext("consts", [128, 6, 128])
        neg_d = ext("negm", [128, 128])

        win_b = P.dram("win_b", [NL, D, INC], BF16)
        wo_b = P.dram("wo_b", [NL, 8, 128, 8, 128], BF16)
        wg_b = P.dram("wg_b", [NL, NFF // 2, 128, 8, 256], BF16)
        wu_b = P.dram("wu_b", [NL, NFF // 2, 128, 8, 256], BF16)
        wd_b = P.dram("wd_b", [NL, DFF, D], BF16)
        for (dst, src) in ((wg_b, wg_d), (wu_b, wu_d)):
            for l in (layers if not os.environ.get('KNOCONV') else []):
                for f2 in range(NFF // 2):
                    P.dma("pool", V(dst.ap[l, f2], dst.buf),
                          V(src.ap[l, :, f2 * 256:(f2 + 1) * 256].rearrange("(k p) c -> p k c", p=128), src.buf))
        for l in (layers if not os.environ.get('KNOCONV') else []):
            for co in range(8):
                P.dma("pool", V(wo_b.ap[l, co], wo_b.buf),
                      V(wo_d.ap[l, :, co * 128:(co + 1) * 128].rearrange("(k p) c -> p k c", p=128), wo_d.buf))
        for (dst, src, rows) in ((win_b, win_d, D), (wd_b, wd_d, DFF)):
            for l in (layers if not os.environ.get('KNOCONV') else []):
                for r0 in range(0, rows, 512):
                    r1 = min(rows, r0 + 512)
                    P.dma("pool", V(dst.ap[l, r0:r1, :], dst.buf), V(src.ap[l, r0:r1, :], src.buf))

        cst = P.sb([128, 6, 128], BF16, "cst")
        P.dma("pool", cst, cst_d)
        ident, m_le, m_lt, m_gt, tinc, bones = (cst[:, i, :] for i in range(6))
        ident_f = P.sb([128, 128], F32, "identf")
        P.dma("sp", ident_f, V(cst_d.ap[:, 0, :], cst_d.buf))
        negm = P.sb([128, 128], F32, "negm")
        P.dma("sp", negm, neg_d)
        cosT = P.sb([128, NB, 32], F32, "cos")
        sinT = P.sb([128, NB, 32], F32, "sin")
        P.dma("sp", cosT, cos_d)
        P.dma("sp", sinT, sin_d)
        ones_bf = P.sb([128, 128], BF16, "ones")
        P.memset("dve", ones_bf, 1.0)
        bandm = P.sb([128, 2, 128], BF16, "bandm")
        P.copy("dve", bandm[:, 0, :], m_gt)
        P.copy("dve", bandm[:, 1, :], m_le)
        epsb = P.sb([128, 1], F32, "epsb")
        P.memset("dve", epsb, EPS)
        onesb = P.sb([128, 1], F32, "onesb")
        P.memset("dve", onesb, 1.0)
        gains = P.sb([128, NL, 4, HD], F32, "gains")
        P.dma("sp", gains, gains_d)
        for l in (range(NL) if 'g' not in SKIP else []):
            P.ts("dve", gains[:, l, 0, :], gains[:, l, 0, :], 0.125, ALU.mult)
            P.ts("dve", gains[:, l, 2, :], gains[:, l, 2, :], 0.125, ALU.mult)
        esink = P.sb([128, NL, 4], F32, "esink")
        P.dma("sp", esink, sinks_d)
        if 'e' not in SKIP: P.act(esink, esink, AF.Exp)
        gout = P.sb([128, NL, 8], F32, "gout")
        P.dma("sp", gout, gout_d)
        ln1 = P.sb([128, NL, 8], F32, "ln1")
        ln2 = P.sb([128, NL, 8], F32, "ln2")
        P.dma("sp", ln1, ln1_d)
        P.dma("sp", ln2, ln2_d)
        bmod = P.sb([128, NL, 48], F32, "bmod")
        P.dma("sp", bmod, bmod_d)
        cact = P.sb([128, 8, NBC], F32, "cact")
        P.dma("sp", cact, cT_d)
        if 's' not in SKIP: P.act(cact, cact, AF.Silu)
        mod = P.sb([128, NL, 48, NBC], F32, "mod")
        A1 = P.sb([128, NL, 8, NBC], F32, "A1")
        A2 = P.sb([128, NL, 8, NBC], F32, "A2")

        PS = [P.ps(f"ps{i}") for i in range(8)]
        rr = {}

        def psum(pool):
            k = rr.get(pool, 0)
            rr[pool] = (k + 1) % len(pool)
            return PS[pool[k]]

        xT = [[P.sb([128, 512], F32, f"x{c}_{t}") for t in range(NT)] for c in range(8)]
        rstd = P.sb([128, 512], F32, "rstd")
        hT = [P.sb([128, 512], BF16, f"h{c}") for c in range(8)]
        sqt = [P.sb([128, 512], BF16, f"sq{i}") for i in range(2)]
        tmpf = [P.sb([128, 512], F32, f"tmpf{i}") for i in range(2)]

        ARENA = 113600
        P.arena_init(ARENA)

        wm = [P.ar([128, 8, 128], F32, f"wm{i}") for i in range(2)]
        it = 0
        for l in (layers if not os.environ.get('KNOMOD') else []):
            for j in range(48):
                w = wm[it % 2]
                it += 1
                P.dma("sp", w, V(wmod_d.ap[l, :, j * 128:(j + 1) * 128].rearrange("(k p) c -> p k c", p=128), wmod_d.buf))
                pm = psum((0, 1))
                for k in range(8):
                    P.mm(pm[:, 0:NBC], w[:, k, :], cact[:, k, :], start=(k == 0), stop=(k == 7))
                P.ts("dve", mod[:, l, j, :], pm[:, 0:NBC], bmod[:, l, j:j + 1], ALU.add)
        for l in (layers if 'a' not in SKIP else []):
            for c in range(8):
                P.ts("dve", A1[:, l, c, :], mod[:, l, 8 + c, :], 1.0, ALU.add, ln1[:, l, c:c + 1], ALU.mult)
                P.ts("dve", A2[:, l, c, :], mod[:, l, 32 + c, :], 1.0, ALU.add, ln2[:, l, c:c + 1], ALU.mult)
        P.barrier()

        def B1(l, c, b): return mod[:, l, 0 + c, b:b + 1]
        def G1(l, c, b): return mod[:, l, 16 + c, b:b + 1]
        def B2(l, c, b): return mod[:, l, 24 + c, b:b + 1]
        def G2(l, c, b): return mod[:, l, 40 + c, b:b + 1]

        P.arena_reset()
        kT = [P.ar([128, 2, 512], BF16, f"kT{t}") for t in range(NT)]
        kbT = [[P.ar([128, 512], BF16, f"kb{p}_{t}") for t in range(NT)] for p in range(2)]
        vall = [P.ar([128, 4, 448], BF16, f"v{t}") for t in range(NT)]
        qT = P.ar([128, 8, 512], BF16, "qT")
        qbT = [P.ar([128, 512], BF16, f"qb{p}") for p in range(2)]
        nqbT = [P.ar([128, 512], BF16, f"nqb{p}") for p in range(2)]
        wi = P.ar([128, 4, 4], F32, "wi")
        u_sq, u_t1 = tmpf
        u_ys = [P.ar([128, 512], F32, f"u_y{i}") for i in range(2)]
        u_rs = [P.ar([128, 512], F32, f"u_r{i}") for i in range(2)]
        u_os = [P.ar([128, 512], BF16, f"u_o{i}") for i in range(2)]
        st_sss = [P.ar([128, 8], F32, f"st_ss{i}") for i in range(2)]
        st_rss = [P.ar([128, 8], F32, f"st_rs{i}") for i in range(2)]
        rp = {"i": 0}
        wA = P.ar([128, 8, 512], BF16, "wA")
        wB = P.ar([128, 8, 260], BF16, "wB")
        wo_s = [P.ar([128, 8, 128], BF16, f"wo{i}") for i in range(2)]
        sc = P.ar([128, L], F32, "sc")
        rel = [P.ar([128, 512], BF16, f"rel{i}") for i in range(2)]
        msk = P.ar([128, L], BF16, "msk")
        junk = P.ar([128, L], U8, "junk")
        mT = P.ar([128, NB, 512], U8, "mT")
        bs = {k: P.ar([128, 1], F32, "bs_" + k) for k in ("lo", "w0", "mid", "cnt", "t", "hi")}
        pT = [P.ar([128, 512], BF16, f"pT{i}") for i in range(3)]
        spb = [P.ar([128, 512], BF16, f"sp{i}") for i in range(2)]
        Rb = P.ar([128, 512], BF16, "Rb")
        on = [u_ys[0], u_rs[0]]
        rden = P.ar([128, 512], F32, "rden")
        ef = rden
        oT = [P.ar([128, 512], BF16, f"o{c}") for c in range(8)]
        att_bytes = P.arena_off
        P.arena_reset()
        actT = [P.ar([128, 512], BF16, f"a{f}") for f in range(NFF)]
        sg = [P.ar([128, 512], F32, f"sg{i}") for i in range(2)]
        wgu = [P.ar([128, 8, 2, 256], BF16, f"wgu{i}") for i in range(2)]
        wdn = [P.ar([128, 2, 1024], BF16, f"wdn{i}") for i in range(2)]
        xstages = [P.ar([128, 1024], F32, f"xstage{i}") for i in range(2)]

        def wslice(wb, l, cols0, ncols):
            return V(wb.ap[l, :, cols0:cols0 + ncols].rearrange("(k p) c -> p k c", p=128), wb.buf)

        def norm_stats(t):
            pm = psum((0, 1))
            for c in range(8):
                s = sqt[c % 2]
                P.act(s, xT[c][t], AF.Square)
                P.mm(pm, ones_bf, s, start=(c == 0), stop=(c == 7))
            P.act(tmpf[0], pm, AF.Sqrt, scale=1.0 / D, bias=epsb)
            P.recip(rstd, tmpf[0])

        def make_h(t, A, Bf, l, b):
            norm_stats(t)
            for c in range(8):
                tm = tmpf[c % 2]
                P.tt("pool", tm, xT[c][t], rstd, ALU.mult)
                P.act(hT[c], tm, AF.Identity, scale=A[:, l, c, b:b + 1], bias=Bf(l, c, b))

        def hview(v, w, perm):
            nh = w // 64
            if perm:
                return V(v.ap[:, :w].rearrange("p (i s two d) -> p s i two d", s=2, two=2, d=32), v.buf)
            return V(v.ap[:, :w].rearrange("p (s i two d) -> p s i two d", s=2, two=2, d=32), v.buf)

        def rope_norm(pu, nh, tb_glob, gsegs, perm):
            w = nh * 64
            rp["i"] ^= 1
            u_y, u_r, u_o, st_ss, st_rs = u_ys[rp["i"]], u_rs[rp["i"]], u_os[rp["i"]], st_sss[rp["i"]], st_rss[rp["i"]]
            if any(g is not None for (_, _, g) in gsegs):
                P.act(u_sq[:, :w], pu[:, :w], AF.Square)
                P.reduce(st_ss[:, :nh], V(u_sq.ap[:, :w].rearrange("p (h d) -> p h d", d=64), u_sq.buf), ALU.add)
                P.act(st_rs[:, :nh], st_ss[:, :nh], AF.Sqrt, scale=1.0 / HD, bias=epsb)
                P.recip(st_rs[:, :nh], st_rs[:, :nh])
            for (h0, h1, g) in gsegs:
                n = h1 - h0
                src = V(pu.ap[:, h0 * 64:h1 * 64].rearrange("p (h d) -> p h d", d=64), pu.buf)
                dst = V(u_y.ap[:, h0 * 64:h1 * 64].rearrange("p (h d) -> p h d", d=64), u_y.buf)
                if g is None:
                    P.copy("act", dst, src)
                else:
                    P.tt("dve", dst, src, V(st_rs.ap[:, h0:h1].unsqueeze(2).to_broadcast([128, n, 64]), st_rs.buf), ALU.mult)
                    P.tt("pool", dst, dst, V(g.ap.unsqueeze(1).to_broadcast([128, n, 64]), g.buf), ALU.mult)
            y5 = hview(u_y, w, False)
            t5 = hview(u_t1, w, False)
            r5 = hview(u_r, w, False)
            o5 = hview(u_o, w, perm)
            hh = nh // 2
            cb = V(cosT.ap[:, tb_glob, :].unsqueeze(1).unsqueeze(1).unsqueeze(1).to_broadcast([128, 2, hh, 2, 32]), cosT.buf)
            sb_ = V(sinT.ap[:, tb_glob, :].unsqueeze(1).unsqueeze(1).to_broadcast([128, 2, hh, 32]), sinT.buf)
            for s_ in range(2):
                P.tt("dve", t5[:, s_], y5[:, s_], cb[:, s_], ALU.mult)
            P.tt("pool", r5[:, :, :, 0, :], y5[:, :, :, 1, :], sb_, ALU.mult)
            P.tt("pool", r5[:, :, :, 1, :], y5[:, :, :, 0, :], sb_, ALU.mult)
            P.tt("dve", o5[:, :, :, 0, :], t5[:, :, :, 0, :], r5[:, :, :, 0, :], ALU.subtract)
            P.tt("dve", o5[:, :, :, 1, :], t5[:, :, :, 1, :], r5[:, :, :, 1, :], ALU.add)
            return u_o

        def transpose_to(dst3, nchunks, u_o):
            ptr = psum((6, 7))
            ptb = V(ptr.ap.bitcast(BF16), ptr.buf)
            for i in range(nchunks):
                P.tr(ptb[:, i * 128:(i + 1) * 128], u_o[:, i * 128:(i + 1) * 128], ident)
            P.copy("act", dst3, V(ptb.ap[:, :nchunks * 128].rearrange("p (i q) -> p i q", q=128), ptb.buf))

        def stage_kv(l, b):
            P.dma("sp", wB[:, :, 0:256], wslice(win_b, l, O_KB, 256))
            for t in range(NT):
                make_h(t, A1, B1, l, b)
                for p in range(2):
                    pm = psum((0, 1))
                    for k in range(8):
                        P.mm(pm, wB[:, k, p * 128:(p + 1) * 128], hT[k], start=(k == 0), stop=(k == 7))
                    P.copy("act", kbT[p][t], pm)
                o = 0
                for (c0, n) in ((O_KA, 64), (O_KI, 64), (O_KC, 128)):
                    P.dma("sp", wA[:, :, o:o + n], wslice(win_b, l, c0, n))
                    o += n
                for tb in range(4):
                    pk = psum((2, 3))
                    for k in range(8):
                        P.mm(pk[:, :256], hT[k][:, tb * 128:(tb + 1) * 128], wA[:, k, 0:256], start=(k == 0), stop=(k == 7))
                    uo = rope_norm(pk, 4, t * 4 + tb, [(0, 1, gains[:, l, 1, :]), (1, 2, None), (2, 4, gains[:, l, 3, :])], False)
                    transpose_to(kT[t][:, :, tb * 128:(tb + 1) * 128], 2, uo)
                o = 0
                for (c0, n) in ((O_VA, 64), (O_VB, 256), (O_VC, 128)):
                    P.dma("sp", wA[:, :, o:o + n], wslice(win_b, l, c0, n))
                    o += n
                for tb in range(4):
                    pv = psum((4, 5))
                    for k in range(8):
                        P.mm(pv[:, :448], hT[k][:, tb * 128:(tb + 1) * 128], wA[:, k, 0:448], start=(k == 0), stop=(k == 7))
                    P.copy("act", vall[t][:, tb, :], pv[:, :448])

        def stage_q(l, b, G):
            make_h(G, A1, B1, l, b)
            P.dma("sp", wB[:, :, 0:256], wslice(win_b, l, O_QB, 256))
            P.dma("sp", wB[:, :, 256:260], wslice(win_b, l, O_WI, 4))
            for p in range(2):
                pm = psum((0, 1))
                for k in range(8):
                    P.mm(pm, wB[:, k, p * 128:(p + 1) * 128], hT[k], start=(k == 0), stop=(k == 7))
                P.act(qbT[p], pm, AF.Copy, scale=0.125)
                P.act(nqbT[p], pm, AF.Copy, scale=-0.125)
            for tb in range(4):
                pw = psum((0, 1))
                for k in range(8):
                    P.mm(pw[:, 0:4], hT[k][:, tb * 128:(tb + 1) * 128], wB[:, k, 256:260], start=(k == 0), stop=(k == 7))
                P.act(wi[:, tb, :], pw[:, 0:4], AF.Copy, scale=1.0 / 16.0)
            P.dma("sp", wA[:, :, 0:256], wslice(win_b, l, O_QA, 256))
            P.dma("sp", wA[:, :, 256:512], wslice(win_b, l, O_QI, 256))
            for tb in range(4):
                pa = psum((2, 3))
                for k in range(8):
                    P.mm(pa, hT[k][:, tb * 128:(tb + 1) * 128], wA[:, k, :], start=(k == 0), stop=(k == 7))
                uo = rope_norm(pa, 8, G * 4 + tb, [(0, 4, gains[:, l, 0, :]), (4, 8, None)], True)
                transpose_to(qT[:, 0:4, tb * 128:(tb + 1) * 128], 4, uo)
            P.dma("sp", wA, wslice(win_b, l, O_QC, 512))
            for tb in range(4):
                pc = psum((4, 5))
                for k in range(8):
                    P.mm(pc, hT[k][:, tb * 128:(tb + 1) * 128], wA[:, k, :], start=(k == 0), stop=(k == 7))
                uo = rope_norm(pc, 8, G * 4 + tb, [(0, 8, gains[:, l, 2, :])], True)
                transpose_to(qT[:, 4:8, tb * 128:(tb + 1) * 128], 4, uo)

        def keyblk(j):
            return j // 4, (j % 4) * 128

        LO = slice(0, 64)
        HI = slice(64, 128)

        def topk_block(G, nn):
            n0 = 4 * G
            n = n0 + nn
            K = (n + 1) * 128
            qsl = slice(nn * 128, (nn + 1) * 128)
            if K <= NSEL:
                for j in range(n + 1):
                    P.copy("pool", mT[:, j, qsl], ones_bf if j < n else m_le)
                return
            for kc in range((K + 511) // 512):
                k0 = kc * 512
                kw = min(512, K - k0)
                for h in range(4):
                    pm = PS[2 + h % 2]
                    P.mm(pm[:, :kw], qT[HI, h, qsl], kT[kc][HI, 0, 0:kw])
                    r = rel[h % 2]
                    P.act(r[:, :kw], pm[:, :kw], AF.Relu)
                    if h == 0:
                        P.ts("dve", sc[:, k0:k0 + kw], r[:, :kw], wi[:, nn, 0:1], ALU.mult)
                    else:
                        P.stt(sc[:, k0:k0 + kw], r[:, :kw], wi[:, nn, h:h + 1], sc[:, k0:k0 + kw], ALU.mult, ALU.add)
            P.reduce(bs["hi"], sc[:, :K], ALU.max)
            P.reduce(bs["lo"], sc[:, :K], ALU.min)
            P.tt("dve", bs["w0"], bs["hi"], bs["lo"], ALU.subtract)
            P.tt("dve", sc[:, n * 128:K], sc[:, n * 128:K], negm, ALU.add)
            for i in range(1, NIT + 1):
                f = 2.0 ** (-i)
                P.stt(bs["mid"], bs["w0"], f, bs["lo"], ALU.mult, ALU.add)
                P.ts("dve", junk[:, :K], sc[:, :K], bs["mid"], ALU.is_ge, 0.0, ALU.add, accum=bs["cnt"])
                P.ts("dve", bs["t"], bs["cnt"], float(NSEL), ALU.is_ge, f, ALU.mult)
                P.stt(bs["lo"], bs["t"], bs["w0"], bs["lo"], ALU.mult, ALU.add)
            P.ts("dve", msk[:, :K], sc[:, :K], bs["lo"], ALU.is_ge)

        def mask_transposes(G, nn):
            n = 4 * G + nn
            K = (n + 1) * 128
            qsl = slice(nn * 128, (nn + 1) * 128)
            if K <= NSEL:
                return
            for j0 in range(0, n + 1, 8):
                j1 = min(n + 1, j0 + 8)
                ptr = psum((6, 7))
                ptb = V(ptr.ap.bitcast(BF16), ptr.buf)
                for j in range(j0, j1):
                    P.tr(ptb[:, (j - j0) * 128:(j - j0 + 1) * 128], msk[:, j * 128:(j + 1) * 128], ident)
                P.copy("act", mT[:, j0:j1, qsl],
                       V(ptb.ap[:, :(j1 - j0) * 128].rearrange("p (i q) -> p i q", q=128), ptb.buf))

        def norm_heads(l, chunks):
            for i, c in enumerate(chunks):
                s = sqt[i % 2]
                P.act(s, on[i], AF.Square)
                pm = psum((6, 7))
                P.mm(pm, bones, s)
                P.act(rden, pm, AF.Sqrt, scale=1.0 / HD, bias=epsb)
                P.recip(rden, rden)
                P.stt(oT[c], on[i], gout[:, l, c:c + 1], rden, ALU.mult, ALU.mult)

        def dsa(G, l):
            n0 = 4 * G
            jmax = n0 + 3
            po = [PS[0], PS[1]]
            pd = [PS[2], PS[3]]
            tiles = [(j, h) for j in range(jmax + 1) for h in range(4)]

            def geom(j):
                qs = max(0, j - n0) * 128
                tk, ko = keyblk(j)
                return qs, 512 - qs, tk, ko

            def stage1(i):
                j, h = tiles[i]
                qs, N, tk, ko = geom(j)
                pz = psum((4, 5))
                P.mm(pz[:, :N], kT[tk][LO, 0, ko:ko + 128], qT[LO, h, qs:512])
                pt = pT[i % 3]
                P.act(pt[:, :N], pz[:, :N], AF.Exp)
                P.tt("pool", pt[:, :N], pt[:, :N], mT[:, j, qs:512], ALU.mult)

            def stage2(i):
                j, h = tiles[i]
                qs, N, tk, ko = geom(j)
                pr, half = h // 2, h % 2
                hs = HI if half else LO
                pt = pT[i % 3]
                P.mm(po[pr][hs, qs:512], vall[tk][:, j % 4, 0:64], pt[:, :N], start=(j == 0), stop=(j == jmax))
                P.mm(pd[pr][hs, qs:512], ones_bf[:, 0:64], pt[:, :N], start=(j == 0), stop=(j == jmax))

            nt_ = len(tiles)
            for i in range(nt_ + 1):
                if i < nt_:
                    stage1(i)
                if i >= 1:
                    stage2(i - 1)
            for pr in range(2):
                P.recip(rden, pd[pr])
                P.tt("dve", on[pr], po[pr], rden, ALU.mult)
            norm_heads(l, [0, 1])

        def sb_head(G, h):
            n0 = 4 * G
            jmax = n0 + 3
            po = [PS[0], PS[1]]
            pr, half = h // 2, h % 2
            hs = HI if half else LO
            P.memset("pool", Rb, 0.0)

            def geom(j):
                qs = max(0, j - n0) * 128
                tk, ko = keyblk(j)
                return qs, 512 - qs, tk, ko, j >= n0

            def stage1(j):
                qs, N, tk, ko, diag = geom(j)
                pz = psum((4, 5))
                P.mm(pz[:, :N], kbT[pr][tk][hs, ko:ko + 128], qbT[pr][hs, qs:512])
                s = spb[j % 2]
                P.act(ef[:, :N], pz[:, :N], AF.Exp)
                P.act(s[:, :N], ef[:, :N], AF.Ln, bias=onesb)
                if diag:
                    P.tt("pool", s[:, 0:128], s[:, 0:128], m_lt, ALU.mult)

            def stage2(j):
                qs, N, tk, ko, diag = geom(j)
                s = spb[j % 2]
                pc = psum((6, 7))
                P.mm(pc[:, :N], kbT[pr][tk][hs, ko:ko + 128], nqbT[pr][hs, qs:512], start=True, stop=False)
                P.mm(pc[:, :N], tinc, s[:, :N], start=False, stop=(j == jmax))
                if j < jmax:
                    P.mm(pc[:, :N], ones_bf, Rb[:, qs:512], start=False, stop=True)
                a = pT[j % 3]
                P.act(a[:, :N], pc[:, :N], AF.Exp, scale=-1.0)
                if diag:
                    P.tt("pool", a[:, 0:128], a[:, 0:128], m_lt, ALU.mult)
                if j > 0:
                    P.tt("pool", Rb[:, qs:512], Rb[:, qs:512], s[:, :N], ALU.add)

            def stage3(j):
                qs, N, tk, ko, diag = geom(j)
                P.mm(po[pr][hs, qs:512], vall[tk][:, j % 4, 64 + h * 64:64 + (h + 1) * 64], pT[j % 3][:, :N],
                     start=(j == jmax), stop=(j == 0))

            for step in range(jmax, -3, -1):
                if step >= 0:
                    stage1(step)
                if 0 <= step + 1 <= jmax:
                    stage2(step + 1)
                if 0 <= step + 2 <= jmax:
                    stage3(step + 2)

        def sb_finish(l):
            for pr in range(2):
                P.copy("dve", on[pr], PS[pr])
            norm_heads(l, [2, 3])

        def swa(G, l):
            n0 = 4 * G
            for g in range(2):
                gs = HI if g else LO
                po = [PS[0], PS[1]]
                pd = [PS[2], PS[3]]
                for nn in range(4):
                    n = n0 + nn
                    kbs = [n - 1, n] if n > 0 else [n]
                    pz = [PS[4], PS[5]]
                    pts = {}
                    for kb in kbs:
                        i = kb - n + 1
                        tk, ko = keyblk(kb)
                        P.mm(V(pz[i].ap.rearrange("p (c q) -> p c q", q=128), pz[i].buf),
                             kT[tk][gs, 1, ko:ko + 128], qT[gs, 4:8, nn * 128:(nn + 1) * 128])
                        pt = pT[(nn * 2 + i) % 3]
                        pts[kb] = pt
                        P.act(pt, pz[i], AF.Exp)
                        P.tt("pool", V(pt.ap.rearrange("p (a q) -> p a q", q=128), pt.buf),
                             V(pt.ap.rearrange("p (a q) -> p a q", q=128), pt.buf),
                             V(bandm.ap[:, i, :].unsqueeze(1).to_broadcast([128, 4, 128]), bandm.buf), ALU.mult)
                    bank = nn // 2
                    off = (nn % 2) * 256
                    for half in range(2):
                        hs = HI if half else LO
                        for kb in kbs:
                            tk, ko = keyblk(kb)
                            rhs = V(pts[kb].ap.rearrange("p (c s q) -> p s c q", s=2, q=128)[:, half], pts[kb].buf)
                            outv = V(po[bank].ap[hs, off:off + 256].rearrange("p (c q) -> p c q", q=128), po[bank].buf)
                            outd = V(pd[bank].ap[hs, off:off + 256].rearrange("p (c q) -> p c q", q=128), pd[bank].buf)
                            P.mm(outv, vall[tk][:, kb % 4, 320 + g * 64:320 + (g + 1) * 64], rhs,
                                 start=(kb == kbs[0]), stop=(kb == kbs[-1]))
                            P.mm(outd, ones_bf[:, 0:64], rhs, start=(kb == kbs[0]), stop=(kb == kbs[-1]))
                for cc in range(2):
                    ch = 2 * g + cc
                    for bank in range(2):
                        den = V(pd[bank].ap.rearrange("p (n c q) -> p n c q", n=2, c=2)[:, :, cc, :], pd[bank].buf)
                        num = V(po[bank].ap.rearrange("p (n c q) -> p n c q", n=2, c=2)[:, :, cc, :], po[bank].buf)
                        rd = V(rden.ap[:, 0:256].rearrange("p (n q) -> p n q", q=128), rden.buf)
                        P.ts("dve", rd, den, esink[:, l, ch:ch + 1], ALU.add)
                        P.recip(rd, rd)
                        dst = V(on[cc].ap[:, bank * 256:(bank + 1) * 256].rearrange("p (n q) -> p n q", q=128), on[cc].buf)
                        P.tt("dve", dst, num, rd, ALU.mult)
                norm_heads(l, [4 + 2 * g, 5 + 2 * g])

        def out_proj(l, b, G):
            for co in range(8):
                w = wo_s[co % 2]
                P.dma("sp", w, V(wo_b.ap[l, co], wo_b.buf))
                pm = psum((4, 5))
                for k in range(8):
                    P.mm(pm, w[:, k, :], oT[k], start=(k == 0), stop=(k == 7))
                P.stt(xT[co][G], pm, G1(l, co, b), xT[co][G], ALU.mult, ALU.add)

        def ffn(l, b):
            for t in range(NT):
                make_h(t, A2, B2, l, b)
                for f2 in range(NFF // 2):
                    w = wgu[f2 % 2]
                    P.dma("sp", w[:, :, 0, :], V(wg_b.ap[l, f2], wg_b.buf))
                    P.dma("sp", w[:, :, 1, :], V(wu_b.ap[l, f2], wu_b.buf))
                    for ff in range(2):
                        f = f2 * 2 + ff
                        pg = psum((0, 1))
                        pu = psum((2, 3))
                        for k in range(8):
                            P.mm(pg, w[:, k, 0, ff * 128:(ff + 1) * 128], hT[k], start=(k == 0), stop=(k == 7))
                        for k in range(8):
                            P.mm(pu, w[:, k, 1, ff * 128:(ff + 1) * 128], hT[k], start=(k == 0), stop=(k == 7))
                        P.act(sg[f % 2], pg, AF.Silu)
                        P.tt("dve", actT[f], sg[f % 2], pu, ALU.mult)
                for f2 in range(NFF // 2):
                    w = wdn[f2 % 2]
                    P.dma("sp", w, V(wd_b.ap[l, f2 * 256:(f2 + 1) * 256, :].rearrange("(k p) c -> p k c", p=128), wd_b.buf))
                    for ff in range(2):
                        f = f2 * 2 + ff
                        for co in range(8):
                            P.mm(PS[co], w[:, ff, co * 128:(co + 1) * 128], actT[f], start=(f == 0), stop=(f == NFF - 1))
                for co in range(8):
                    P.stt(xT[co][t], PS[co], G2(l, co, b), xT[co][t], ALU.mult, ALU.add)

        for b in range(NBC):
            P.barrier()
            for t in (range(NT) if 'x' not in SKIP else []):
                for tb in range(4):
                    r0 = (t * 4 + tb) * 128
                    xstage = xstages[tb % 2]
                    P.dma("sp", xstage, V(x_d.ap[b, r0:r0 + 128, :], x_d.buf))
                    for c4 in range(0, 8, 4):
                        pm = psum((0, 1, 2, 3))
                        for c in range(c4, c4 + 4):
                            P.tr(pm[:, (c - c4) * 128:(c - c4 + 1) * 128], xstage[:, c * 128:(c + 1) * 128], ident_f)
                        for c in range(c4, c4 + 4):
                            P.copy("act" if c % 2 else "dve", xT[c][t][:, tb * 128:(tb + 1) * 128],
                                   pm[:, (c - c4) * 128:(c - c4 + 1) * 128])
            for l in layers:
                P.barrier()
                if STOP < 1: continue
                stage_kv(l, b)
                for G in range(NT):
                    if STOP < 2: continue
                    stage_q(l, b, G)
                    if STOP < 3: continue
                    for nn in range(4):
                        topk_block(G, nn)
                        sb_head(G, nn)
                        mask_transposes(G, nn)
                    sb_finish(l)
                    swa(G, l)
                    dsa(G, l)
                    out_proj(l, b, G)
                P.barrier()
                if STOP < 8: continue
                ffn(l, b)
            P.barrier()
            for t in range(NT):
                for tb in range(4):
                    r0 = (t * 4 + tb) * 128
                    xstage = xstages[tb % 2]
                    for c4 in (range(0, 8, 4) if 'y' not in SKIP else []):
                        pm = psum((0, 1, 2, 3))
                        for c in range(c4, c4 + 4):
                            P.tr(pm[:, (c - c4) * 128:(c - c4 + 1) * 128], xT[c][t][:, tb * 128:(tb + 1) * 128], ident_f)
                        P.copy("act" if c4 else "dve", xstage[:, c4 * 128:(c4 + 4) * 128], pm)
                    P.dma("sp", V(y_d.ap[b, r0:r0 + 128, :], y_d.buf), xstage, out_is_ext=True)

        print("arena bytes: att", att_bytes, "max", P.arena_max, "ops", {k: len(v) for k, v in P.ops.items()})
        block = es.enter_context(nc.Block())
        P.emit(block)
    return nc


_CACHE = {}


def _host_consts(L):
    NB = L // 128
    kk = np.arange(128)[:, None]
    qq = np.arange(128)[None, :]
    cst = np.zeros((128, 6, 128), np.float32)
    cst[:, 0] = np.eye(128)
    cst[:, 1] = (kk <= qq)
    cst[:, 2] = (kk < qq)
    cst[:, 3] = (kk > qq)
    cst[:, 4] = (kk >= qq)
    cst[:, 5] = ((kk // 64) == (qq // 64))
    negm = np.where(qq <= kk, 0.0, NEG).astype(np.float32)
    inv = (1.0 / (np.float32(10000.0) ** (np.arange(0, 64, 2, dtype=np.float32) / np.float32(64)))).astype(np.float32)
    ang = np.arange(L, dtype=np.float32)[:, None] * inv[None, :]
    cos = np.cos(ang).astype(np.float32).reshape(NB, 128, 32).transpose(1, 0, 2)
    sin = np.sin(ang).astype(np.float32).reshape(NB, 128, 32).transpose(1, 0, 2)
    return cst, negm, np.ascontiguousarray(cos), np.ascontiguousarray(sin)


def _run(inputs, L, NBC, ncores, layers):
    key = (L, NBC, tuple(layers))
    if key not in _CACHE:
        _CACHE[key] = build_program(L, NBC, layers)
    nc = _CACHE[key]
    f = lambda a: np.ascontiguousarray(np.asarray(a, dtype=np.float32))
    cst, negm, cos, sin = _host_consts(L)
    x = f(inputs["x"])
    c = f(inputs["c"])
    NL = 4
    def featT(a):
        a = f(a)
        return np.ascontiguousarray(a.reshape(NL, -1, 128).transpose(2, 0, 1))
    gains = np.stack([f(inputs["qn_a"]), f(inputs["kn_a"]), f(inputs["qn_c"]), f(inputs["kn_c"])], axis=1)
    gainsB = np.ascontiguousarray(np.broadcast_to(gains[None], (128, NL, 4, 64)))
    sk = f(inputs["sinks"])
    sinksT = np.zeros((128, NL, 4), np.float32)
    for ch in range(4):
        sinksT[:64, :, ch] = sk[:, ch][None, :]
        sinksT[64:, :, ch] = sk[:, 4 + ch][None, :]
    common = {
        "ln1T": featT(inputs["ln1"]), "ln2T": featT(inputs["ln2"]), "bmodT": featT(inputs["b_mod"]),
        "w_mod": f(inputs["w_mod"]), "w_in": f(inputs["w_in"]), "w_o": f(inputs["w_o"]),
        "w_gate": f(inputs["w_gate"]), "w_up": f(inputs["w_up"]), "w_down": f(inputs["w_down"]),
        "gainsB": gainsB, "sinksT": sinksT, "goutT": featT(f(inputs["g_out"]).reshape(NL, -1)),
        "cosT": cos, "sinT": sin, "consts": cst, "negm": negm,
    }
    in_maps = []
    for i in range(ncores):
        cb = c[i * NBC:(i + 1) * NBC]
        cT = np.ascontiguousarray(cb.reshape(NBC, 8, 128).transpose(2, 1, 0))
        m = dict(common)
        m["x"] = np.ascontiguousarray(x[i * NBC:(i + 1) * NBC])
        m["cT"] = cT
        in_maps.append(m)
    res = run_bass_kernel_spmd(nc, in_maps, core_ids=list(range(ncores)))
    return np.concatenate([r["y"] for r in res.results], axis=0)


def kernel(x, c, ln1, ln2, w_mod, b_mod, w_in, qn_a, kn_a, qn_c, kn_c, sinks, g_out, w_o, w_gate, w_up, w_down):
    inputs = dict(x=x, c=c, ln1=ln1, ln2=ln2, w_mod=w_mod, b_mod=b_mod, w_in=w_in, qn_a=qn_a, kn_a=kn_a,
                  qn_c=qn_c, kn_c=kn_c, sinks=sinks, g_out=g_out, w_o=w_o, w_gate=w_gate, w_up=w_up, w_down=w_down)
    B, L = x.shape[0], x.shape[1]
    out = _run(inputs, L, B // 8, 8, [0, 1, 2, 3])
    return out.astype(np.float32)
```
